# Optimizing a Trainium2 kernel written in Bass

```python
import jax
import jax.numpy as jnp
from jax import lax
import numpy as np

D_MODEL = 2048
BATCH = 8
SEQ = 4096
DEPTH = 4

GRID_W = 64
CTX_LEN = 256
EPS = 1e-6
N_MOD = 6
N_BRANCH = 4

MLA_HEADS = 8
MLA_NOPE = 128
MLA_ROPE = 64
MLA_V = 128
Q_LORA = 512
KV_LORA = 512
ROPE_BASE = 10000.0
Q_BLOCK = 128

NA_HEADS = 8
NA_DIM = 128
WIN_R = 8
WIN_C = 16

LRU_W = 1024
LRU_BLOCKS = 8
LRU_C = 8.0
CONV_W = 4
CONV_PAD_L = 2

DN_HEADS = 8
DN_DK = 128
DN_DV = 128
DN_CHUNK = 64

N_EXPERTS = 32
TOP_K = 4
D_FF = 640
SWIGLU_ALPHA = 1.702
SWIGLU_LIMIT = 7.0

MLA_W = MLA_HEADS * MLA_V
NA_W = NA_HEADS * NA_DIM
DN_W = DN_HEADS * DN_DV
DN_QKV = DN_HEADS * (2 * DN_DK + DN_DV)
BRANCH_W = 1024
IN_SPLITS = (Q_LORA, KV_LORA, MLA_ROPE, NA_W, NA_W, NA_W, LRU_W, LRU_W, DN_QKV, DN_W, 2 * DN_HEADS, 2 * DN_HEADS, N_BRANCH * D_MODEL)
IN_COLS = Q_LORA + KV_LORA + MLA_ROPE + 3 * NA_W + 2 * LRU_W + DN_QKV + DN_W + 4 * DN_HEADS + N_BRANCH * D_MODEL

kernel_name = 'hybrid_diffusion_trunk'


def rms_norm(x, g):
    xf = x.astype(jnp.float32)
    y = xf * lax.rsqrt(jnp.mean(xf * xf, axis=-1, keepdims=True) + EPS)
    return (y * g.astype(jnp.float32)).astype(x.dtype)


def l2_normalize(x):
    xf = x.astype(jnp.float32)
    return (xf * lax.rsqrt(jnp.sum(xf * xf, axis=-1, keepdims=True) + EPS)).astype(x.dtype)


def modulate(h, shift, scale):
    return h * (1.0 + scale) + shift


def to_heads(t, n_heads):
    return t.reshape(t.shape[0], t.shape[1], n_heads, t.shape[2] // n_heads)


def flip_seq(ts, rev):
    return tuple(jnp.flip(t, axis=1) for t in ts) if rev else tuple(ts)


def split_columns(p):
    parts, start = [], 0
    for size in IN_SPLITS:
        parts.append(p[..., start:start + size])
        start += size
    return parts


def rope_2d_tables(n, dim):
    t = jnp.arange(n)
    row = (t // GRID_W).astype(jnp.float32)
    col = (t % GRID_W).astype(jnp.float32)
    nf = dim // 4
    inv_freq = ROPE_BASE ** (-jnp.arange(nf, dtype=jnp.float32) / nf)
    ang = jnp.stack([row[:, None] * inv_freq, col[:, None] * inv_freq], axis=1)
    return jnp.cos(ang), jnp.sin(ang)


def apply_rope_2d(x, cos, sin):
    B, n, H, dim = x.shape
    xr = x.reshape(B, n, H, 2, 2, dim // 4)
    a, b = xr[..., 0, :], xr[..., 1, :]
    c, s = cos[None, :, None], sin[None, :, None]
    out = jnp.stack([a * c - b * s, b * c + a * s], axis=-2)
    return out.reshape(x.shape).astype(x.dtype)


def short_conv(x, w, b=None):
    ch = x.shape[-1]
    y = lax.conv_general_dilated(x, w[:, None, :].astype(x.dtype), window_strides=(1,),
                                 padding=[(CONV_PAD_L, CONV_W - 1 - CONV_PAD_L)],
                                 dimension_numbers=('NWC', 'WIO', 'NWC'), feature_group_count=ch)
    if b is not None:
        y = y + b
    return y


def block_softmax_attention(q, k, v):
    B, S, H, dk = q.shape
    nb = S // Q_BLOCK
    scale = dk ** -0.5
    qb = jnp.moveaxis(q.reshape(B, nb, Q_BLOCK, H, dk), 1, 0)

    def one_block(qi):
        s = jnp.einsum('bqhd,bkhd->bhqk', qi, k).astype(jnp.float32) * scale
        p = jax.nn.softmax(s, axis=-1).astype(v.dtype)
        return jnp.einsum('bhqk,bkhd->bqhd', p, v)

    o = lax.map(one_block, qb)
    return jnp.moveaxis(o, 0, 1).reshape(B, S, H, v.shape[-1])


def mla_queries(q_c, qn_g, wq_up, rope):
    B, S, _ = q_c.shape
    q = (rms_norm(q_c, qn_g) @ wq_up).reshape(B, S, MLA_HEADS, MLA_NOPE + MLA_ROPE)
    q_nope, q_pe = q[..., :MLA_NOPE], q[..., MLA_NOPE:]
    if rope is not None:
        q_pe = apply_rope_2d(q_pe, *rope)
    return jnp.concatenate([q_nope, q_pe], axis=-1)


def mla_keys_values(kv_c, k_pe, kvn_g, wkv_up, rope):
    B, S, _ = kv_c.shape
    kv = (rms_norm(kv_c, kvn_g) @ wkv_up).reshape(B, S, MLA_HEADS, MLA_NOPE + MLA_V)
    k_nope, v = kv[..., :MLA_NOPE], kv[..., MLA_NOPE:]
    k_pe = k_pe[:, :, None, :]
    if rope is not None:
        k_pe = apply_rope_2d(k_pe, *rope)
    k_pe = jnp.broadcast_to(k_pe, (B, S, MLA_HEADS, MLA_ROPE))
    return jnp.concatenate([k_nope, k_pe], axis=-1), v


def neighbourhood_attention(q, k, v, k_ctx, v_ctx, rpb):
    B, N, H, d = q.shape
    rows = N // GRID_W
    wr = min(WIN_R, rows)
    n_loc = wr * WIN_C
    scale = d ** -0.5
    col = jnp.arange(GRID_W)
    col_start = jnp.clip(col - WIN_C // 2, 0, GRID_W - WIN_C)
    col_idx = col_start[:, None] + jnp.arange(WIN_C)
    col_off = col_idx - col[:, None] + (WIN_C - 1)
    qg = q.reshape(B, rows, GRID_W, H, d)
    kg = k.reshape(B, rows, GRID_W, H, d)
    vg = v.reshape(B, rows, GRID_W, H, d)

    def one_row(r):
        row_start = jnp.clip(r - wr // 2, 0, rows - wr)
        k_band = lax.dynamic_slice_in_dim(kg, row_start, wr, axis=1)
        v_band = lax.dynamic_slice_in_dim(vg, row_start, wr, axis=1)
        k_nb = k_band[:, :, col_idx]
        v_nb = v_band[:, :, col_idx]
        q_row = lax.dynamic_index_in_dim(qg, r, axis=1, keepdims=False)
        row_off = row_start + jnp.arange(wr) - r + (WIN_R - 1)
        bias = rpb[:, row_off[:, None, None], col_off[None, :, :]]
        s_loc = jnp.einsum('bqhd,brqjhd->bhqrj', q_row, k_nb).astype(jnp.float32) * scale
        s_loc = s_loc + jnp.transpose(bias, (0, 2, 1, 3)).astype(jnp.float32)[None]
        s_ctx = jnp.einsum('bqhd,bkhd->bhqk', q_row, k_ctx).astype(jnp.float32) * scale
        s = jnp.concatenate([s_loc.reshape(B, H, GRID_W, n_loc), s_ctx], axis=-1)
        p = jax.nn.softmax(s, axis=-1).astype(v.dtype)
        p_loc = p[..., :n_loc].reshape(B, H, GRID_W, wr, WIN_C)
        return (jnp.einsum('bhqrj,brqjhd->bqhd', p_loc, v_nb)
                + jnp.einsum('bhqk,bkhd->bqhd', p[..., n_loc:], v_ctx))

    o = lax.map(one_row, jnp.arange(rows))
    return jnp.moveaxis(o, 0, 1).reshape(B, N, H, d)


def rglru_coeffs(u, wa, ba, wx, bx, lam):
    B, S, W = u.shape
    ub = u.reshape(B, S, LRU_BLOCKS, W // LRU_BLOCKS)
    r = jax.nn.sigmoid(jnp.einsum('bsnj,njk->bsnk', ub, wa).reshape(B, S, W) + ba)
    i = jax.nn.sigmoid(jnp.einsum('bsnj,njk->bsnk', ub, wx).reshape(B, S, W) + bx)
    log_a = (-LRU_C * r.astype(jnp.float32)) * jax.nn.softplus(-lam.astype(jnp.float32))
    a = jnp.exp(log_a)
    b = jnp.sqrt(-jnp.expm1(2.0 * log_a)) * (i * u).astype(jnp.float32)
    return a, b


def linear_scan(a, b, h0, reverse):
    def combine(left, right):
        a_l, b_l = left
        a_r, b_r = right
        return a_l * a_r, a_r * b_l + b_r

    a_cum, h = lax.associative_scan(combine, (a, b), axis=1, reverse=reverse)
    return h + a_cum * h0[:, None]


def deltanet_qkv(qkv, conv_w):
    u = jax.nn.silu(short_conv(qkv, conv_w))
    nq = DN_HEADS * DN_DK
    q = l2_normalize(to_heads(u[..., :nq], DN_HEADS)) * (DN_DK ** -0.5)
    k = l2_normalize(to_heads(u[..., nq:2 * nq], DN_HEADS))
    v = to_heads(u[..., 2 * nq:], DN_HEADS)
    return q, k, v


def deltanet_gates(a_raw, b_raw, a_log, dt_bias):
    g = -jnp.exp(a_log.astype(jnp.float32)) * jax.nn.softplus(a_raw.astype(jnp.float32) + dt_bias)
    beta = jax.nn.sigmoid(b_raw.astype(jnp.float32))
    return g, beta


def gated_delta_chunked(q, k, v, g, beta, s0):
    B, S, H, dk = q.shape
    dv = v.shape[-1]
    out_dtype = v.dtype
    C = DN_CHUNK
    n = S // C
    f32 = jnp.float32

    def chunks(t):
        t = t.astype(f32).reshape(B, n, C, H, *t.shape[3:])
        return jnp.moveaxis(t, 3, 1)

    q, k, v, beta = chunks(q), chunks(k), chunks(v), chunks(beta)
    gc = jnp.cumsum(chunks(g), axis=-1)
    causal = jnp.tril(jnp.ones((C, C), bool))
    strict = jnp.tril(jnp.ones((C, C), bool), -1)
    diff = gc[..., :, None] - gc[..., None, :]
    decay = jnp.where(causal, jnp.exp(jnp.where(causal, diff, 0.0)), 0.0)
    kb = k * beta[..., None]
    lower = jnp.where(strict, jnp.einsum('bhnid,bhnjd->bhnij', kb, k) * decay, 0.0)
    eye = jnp.eye(C, dtype=f32)
    t_inv = lax.linalg.triangular_solve(lower + eye, jnp.broadcast_to(eye, lower.shape),
                                        left_side=True, lower=True, unit_diagonal=True)
    w_v = t_inv @ (v * beta[..., None])
    w_k = t_inv @ (kb * jnp.exp(gc)[..., None])
    a_intra = jnp.einsum('bhnid,bhnjd->bhnij', q, k) * decay
    q_dec = q * jnp.exp(gc)[..., None]
    k_dec = k * jnp.exp(gc[..., -1:] - gc)[..., None]
    g_end = jnp.exp(gc[..., -1])

    def step(state, xs):
        wv, wk, qd, kd, a, ge = xs
        v_new = wv - jnp.einsum('bhcd,bhde->bhce', wk, state)
        o = jnp.einsum('bhcd,bhde->bhce', qd, state) + jnp.einsum('bhcj,bhje->bhce', a, v_new)
        state = state * ge[..., None, None] + jnp.einsum('bhcd,bhce->bhde', kd, v_new)
        return state, o

    xs = tuple(jnp.moveaxis(t, 2, 0) for t in (w_v, w_k, q_dec, k_dec, a_intra, g_end))
    s_fin, o = lax.scan(step, s0.astype(f32), xs)
    o = jnp.moveaxis(jnp.moveaxis(o, 0, 2), 1, 3).reshape(B, S, H, dv)
    return o.astype(out_dtype), s_fin


def deltanet_output(o, z, norm_g):
    return (rms_norm(o, norm_g) * jax.nn.silu(to_heads(z, DN_HEADS))).reshape(z.shape)


def merge_branches(branches, gate_logits, w_branch, w_out):
    g = gate_logits.reshape(*gate_logits.shape[:-1], N_BRANCH, D_MODEL)
    merged = jnp.zeros_like(gate_logits[..., :D_MODEL])
    for i, o in enumerate(branches):
        merged = merged + jax.nn.sigmoid(g[..., i, :]) * (o @ w_branch[i])
    return merged @ w_out


def clamped_swiglu(u):
    glu, lin = u[..., ::2], u[..., 1::2]
    glu = jnp.minimum(glu, SWIGLU_LIMIT)
    lin = jnp.clip(lin, -SWIGLU_LIMIT, SWIGLU_LIMIT)
    return glu * jax.nn.sigmoid(SWIGLU_ALPHA * glu) * (lin + 1.0)


def moe_ffn(t, router_w, router_b, w1, b1, w2, b2):
    logits = (t @ router_w + router_b).astype(jnp.float32)
    top_val, top_idx = lax.top_k(logits, TOP_K)
    top_w = jax.nn.softmax(top_val, axis=-1)
    combine = jnp.einsum('tk,tke->te', top_w,
                         jax.nn.one_hot(top_idx, N_EXPERTS, dtype=jnp.float32)).astype(t.dtype)
    out = jnp.zeros_like(t)
    for e in range(N_EXPERTS):
        y = clamped_swiglu(t @ w1[e] + b1[e]) @ w2[e] + b2[e]
        out = out + combine[:, e:e + 1] * y
    return out


def token_mixers(hx, hz, rope, need_ctx, w_in, mla_qn_g, mla_wq_up, mla_kvn_g, mla_wkv_up, na_rpb,
                 lru_conv_w, lru_conv_b, lru_wa, lru_ba, lru_wx, lru_bx, lru_lam,
                 dn_conv_w, dn_a_log, dn_dt_bias, dn_norm_g, w_branch, w_out):
    B, N, _ = hx.shape
    M = hz.shape[1]
    (qc_x, kvc_x, kpe_x, naq_x, nak_x, nav_x, lu_x, ly_x, dqkv_x, dz_x, da_x, db_x, gt_x) = split_columns(hx @ w_in)
    (qc_z, kvc_z, kpe_z, naq_z, nak_z, nav_z, lu_z, ly_z, dqkv_z, dz_z, da_z, db_z, gt_z) = split_columns(hz @ w_in)

    k_x, v_x = mla_keys_values(kvc_x, kpe_x, mla_kvn_g, mla_wkv_up, rope)
    k_z, v_z = mla_keys_values(kvc_z, kpe_z, mla_kvn_g, mla_wkv_up, None)
    q_x = mla_queries(qc_x, mla_qn_g, mla_wq_up, rope)
    o_a_x = block_softmax_attention(q_x, jnp.concatenate([k_x, k_z], axis=1),
                                    jnp.concatenate([v_x, v_z], axis=1)).reshape(B, N, MLA_W)

    nk_z, nv_z = to_heads(nak_z, NA_HEADS), to_heads(nav_z, NA_HEADS)
    o_b_x = neighbourhood_attention(to_heads(naq_x, NA_HEADS), to_heads(nak_x, NA_HEADS),
                                    to_heads(nav_x, NA_HEADS), nk_z, nv_z, na_rpb).reshape(B, N, NA_W)

    u_x = short_conv(lu_x, lru_conv_w, lru_conv_b)
    u_z = short_conv(lu_z, lru_conv_w, lru_conv_b)
    hs_x, hs_z = [], []
    for d, rev in enumerate((False, True)):
        a_z, b_z = rglru_coeffs(u_z, lru_wa[d], lru_ba[d], lru_wx[d], lru_bx[d], lru_lam[d])
        h_z = linear_scan(a_z, b_z, jnp.zeros_like(b_z[:, 0]), rev)
        h_end = h_z[:, 0] if rev else h_z[:, -1]
        a_x, b_x = rglru_coeffs(u_x, lru_wa[d], lru_ba[d], lru_wx[d], lru_bx[d], lru_lam[d])
        hs_x.append(linear_scan(a_x, b_x, h_end, rev))
        hs_z.append(h_z)
    o_c_x = (hs_x[0] + hs_x[1]).astype(hx.dtype) * jax.nn.gelu(ly_x)

    qd_x, kd_x, vd_x = deltanet_qkv(dqkv_x, dn_conv_w)
    qd_z, kd_z, vd_z = deltanet_qkv(dqkv_z, dn_conv_w)
    da_x, db_x = da_x.reshape(B, N, 2, DN_HEADS), db_x.reshape(B, N, 2, DN_HEADS)
    da_z, db_z = da_z.reshape(B, M, 2, DN_HEADS), db_z.reshape(B, M, 2, DN_HEADS)
    od_x, od_z = [], []
    for d, rev in enumerate((False, True)):
        g_z, beta_z = deltanet_gates(da_z[:, :, d], db_z[:, :, d], dn_a_log[d], dn_dt_bias[d])
        g_x, beta_x = deltanet_gates(da_x[:, :, d], db_x[:, :, d], dn_a_log[d], dn_dt_bias[d])
        s_init = jnp.zeros((B, DN_HEADS, DN_DK, DN_DV), jnp.float32)
        o_z, s_z = gated_delta_chunked(*flip_seq((qd_z, kd_z, vd_z, g_z, beta_z), rev), s_init)
        o_x, _ = gated_delta_chunked(*flip_seq((qd_x, kd_x, vd_x, g_x, beta_x), rev), s_z)
        od_x.append(flip_seq((o_x,), rev)[0])
        od_z.append(flip_seq((o_z,), rev)[0])
    o_d_x = deltanet_output(od_x[0] + od_x[1], dz_x, dn_norm_g)

    out_x = merge_branches((o_a_x, o_b_x, o_c_x, o_d_x), gt_x, w_branch, w_out)
    if not need_ctx:
        return out_x, None

    o_a_z = block_softmax_attention(mla_queries(qc_z, mla_qn_g, mla_wq_up, None), k_z, v_z).reshape(B, M, MLA_W)
    o_b_z = block_softmax_attention(to_heads(naq_z, NA_HEADS), nk_z, nv_z).reshape(B, M, NA_W)
    o_c_z = (hs_z[0] + hs_z[1]).astype(hz.dtype) * jax.nn.gelu(ly_z)
    o_d_z = deltanet_output(od_z[0] + od_z[1], dz_z, dn_norm_g)
    out_z = merge_branches((o_a_z, o_b_z, o_c_z, o_d_z), gt_z, w_branch, w_out)
    return out_x, out_z


def setup_inputs(seed: int = 0) -> dict:
    key = jax.random.key(seed)
    ks = iter(jax.random.split(key, 48))
    f32 = jnp.float32
    L, D, E = DEPTH, D_MODEL, N_EXPERTS

    def nrm(shape, scale):
        return jax.random.normal(next(ks), shape, f32) * scale

    def gain(shape):
        return 1.0 + nrm(shape, 0.05)

    a_c = jax.random.uniform(next(ks), (L, 2, LRU_W), f32, 0.9, 0.999)
    a_base = a_c ** (1.0 / LRU_C)
    lru_lam = jnp.log(a_base) - jnp.log1p(-a_base)
    dn_a_log = jnp.log(jax.random.uniform(next(ks), (L, 2, DN_HEADS), f32, 1.0, 16.0))
    return {
        'x': nrm((BATCH, SEQ, D), 1.0),
        'c': nrm((BATCH, D), 1.0),
        'ctx': nrm((BATCH, CTX_LEN, D), 1.0),
        'c_ctx': nrm((D,), 1.0),
        'ada_w': nrm((L, D, N_MOD * D), 0.5 * D ** -0.5),
        'ada_b': nrm((L, N_MOD * D), 0.01),
        'norm_mix_g': gain((L, D)),
        'norm_ffn_g': gain((L, D)),
        'w_in': nrm((L, D, IN_COLS), D ** -0.5),
        'mla_qn_g': gain((L, Q_LORA)),
        'mla_wq_up': nrm((L, Q_LORA, MLA_HEADS * (MLA_NOPE + MLA_ROPE)), Q_LORA ** -0.5),
        'mla_kvn_g': gain((L, KV_LORA)),
        'mla_wkv_up': nrm((L, KV_LORA, MLA_HEADS * (MLA_NOPE + MLA_V)), KV_LORA ** -0.5),
        'na_rpb': nrm((L, NA_HEADS, 2 * WIN_R - 1, 2 * WIN_C - 1), 0.1),
        'lru_conv_w': nrm((L, CONV_W, LRU_W), CONV_W ** -0.5),
        'lru_conv_b': nrm((L, LRU_W), 0.01),
        'lru_wa': nrm((L, 2, LRU_BLOCKS, LRU_W // LRU_BLOCKS, LRU_W // LRU_BLOCKS), (LRU_W // LRU_BLOCKS) ** -0.5),
        'lru_ba': nrm((L, 2, LRU_W), 0.1),
        'lru_wx': nrm((L, 2, LRU_BLOCKS, LRU_W // LRU_BLOCKS, LRU_W // LRU_BLOCKS), (LRU_W // LRU_BLOCKS) ** -0.5),
        'lru_bx': nrm((L, 2, LRU_W), 0.1),
        'lru_lam': lru_lam,
        'dn_conv_w': nrm((L, CONV_W, DN_QKV), CONV_W ** -0.5),
        'dn_a_log': dn_a_log,
        'dn_dt_bias': nrm((L, 2, DN_HEADS), 0.1),
        'dn_norm_g': gain((L, DN_DV)),
        'w_branch': nrm((L, N_BRANCH, BRANCH_W, D), BRANCH_W ** -0.5),
        'w_out': nrm((L, D, D), D ** -0.5),
        'router_w': nrm((L, D, E), D ** -0.5),
        'router_b': nrm((L, E), 0.01),
        'exp_w1': nrm((L, E, D, 2 * D_FF), D ** -0.5),
        'exp_b1': nrm((L, E, 2 * D_FF), 0.01),
        'exp_w2': nrm((L, E, D_FF, D), D_FF ** -0.5),
        'exp_b2': nrm((L, E, D), 0.01),
        'final_g': gain((D,)),
    }


def reference(x, c, ctx, c_ctx, ada_w, ada_b, norm_mix_g, norm_ffn_g, w_in, mla_qn_g, mla_wq_up,
              mla_kvn_g, mla_wkv_up, na_rpb, lru_conv_w, lru_conv_b, lru_wa, lru_ba, lru_wx, lru_bx,
              lru_lam, dn_conv_w, dn_a_log, dn_dt_bias, dn_norm_g, w_branch, w_out, router_w, router_b,
              exp_w1, exp_b1, exp_w2, exp_b2, final_g):
    B, N, D = x.shape
    M = ctx.shape[1]
    rope = rope_2d_tables(N, MLA_ROPE)
    silu_c = jax.nn.silu(c)
    silu_cc = jax.nn.silu(c_ctx)
    z = ctx
    for l in range(DEPTH):
        last = l == DEPTH - 1
        mod_x = (silu_c @ ada_w[l] + ada_b[l]).reshape(B, N_MOD, 1, D)
        mod_z = (silu_cc @ ada_w[l] + ada_b[l]).reshape(N_MOD, D)
        hx = modulate(rms_norm(x, norm_mix_g[l]), mod_x[:, 0], mod_x[:, 1])
        hz = modulate(rms_norm(z, norm_mix_g[l]), mod_z[0], mod_z[1])
        mix_x, mix_z = token_mixers(hx, hz, rope, not last, w_in[l], mla_qn_g[l], mla_wq_up[l],
                                    mla_kvn_g[l], mla_wkv_up[l], na_rpb[l], lru_conv_w[l], lru_conv_b[l],
                                    lru_wa[l], lru_ba[l], lru_wx[l], lru_bx[l], lru_lam[l], dn_conv_w[l],
                                    dn_a_log[l], dn_dt_bias[l], dn_norm_g[l], w_branch[l], w_out[l])
        x = x + mod_x[:, 2] * mix_x
        hx = modulate(rms_norm(x, norm_ffn_g[l]), mod_x[:, 3], mod_x[:, 4])
        if last:
            ffn = moe_ffn(hx.reshape(B * N, D), router_w[l], router_b[l],
                          exp_w1[l], exp_b1[l], exp_w2[l], exp_b2[l])
            x = x + mod_x[:, 5] * ffn.reshape(B, N, D)
        else:
            z = z + mod_z[2] * mix_z
            hz = modulate(rms_norm(z, norm_ffn_g[l]), mod_z[3], mod_z[4])
            tokens = jnp.concatenate([hx.reshape(B * N, D), hz.reshape(B * M, D)], axis=0)
            ffn = moe_ffn(tokens, router_w[l], router_b[l], exp_w1[l], exp_b1[l], exp_w2[l], exp_b2[l])
            x = x + mod_x[:, 5] * ffn[:B * N].reshape(B, N, D)
            z = z + mod_z[5] * ffn[B * N:].reshape(B, M, D)
    return rms_norm(x, final_g)
```

```python
import numpy as np
import concourse.bass as bass
import concourse.mybir as mybir
from contextlib import ExitStack

F32 = mybir.dt.float32
BF16 = mybir.dt.bfloat16
I32 = mybir.dt.int32
U32 = mybir.dt.uint32
AF = mybir.ActivationFunctionType
ALU = mybir.AluOpType
AX = mybir.AxisListType


class Buf:
    __slots__ = ("w", "r", "name")

    def __init__(self, name=""):
        self.w = {}
        self.r = {}
        self.name = name


class Tile(Buf):
    __slots__ = ("t",)

    def __init__(self, t, name=""):
        Buf.__init__(self, name)
        self.t = t

    def __getitem__(self, idx):
        return self.t[idx]


class Eng:
    def __init__(self, name, h, sem):
        self.name = name
        self.h = h
        self.sem = sem
        self.cnt = 0
        self.seen = {}


class DmaQ:
    def __init__(self, name, h, sems):
        self.name = name
        self.h = h
        self.sems = sems
        self.vals = [0] * len(sems)
        self.k = 0
        self.seen = {}


class FW:
    NDMA_SEMS = 8

    def __init__(self, nc):
        self.nc = nc
        self.es = ExitStack()
        self.E = {}
        for name, h in (("pe", nc.tensor), ("act", nc.scalar), ("dve", nc.vector), ("pool", nc.gpsimd)):
            sem = self.es.enter_context(nc.semaphore("s_" + name))
            self.E[name] = Eng(name, h, sem)
        self.Q = {}
        for name, h in (("sp", nc.sync), ("pool", nc.gpsimd), ("act", nc.scalar)):
            sems = [self.es.enter_context(nc.semaphore("d_%s%d" % (name, i))) for i in range(self.NDMA_SEMS)]
            q = DmaQ(name, h, sems)
            if name in self.E:
                q.seen = self.E[name].seen
            self.Q[name] = q
        self.n_instr = 0
        self.uid = 0
        self.scopes = []
        self.bar_sem = self.es.enter_context(nc.semaphore("s_bar"))
        self.bar_cnt = 0

    def sb(self, name, shape, dt):
        self.uid += 1
        es = self.scopes[-1] if self.scopes else self.es
        t = es.enter_context(self.nc.sbuf_tensor("%s_%d" % (name, self.uid), list(shape), dt))
        return Tile(t, name)

    def scope(self):
        fw = self

        class _S:
            def __enter__(s):
                fw.scopes.append(ExitStack())

            def __exit__(s, *a):
                if a[0] is None:
                    fw.barrier()
                fw.scopes.pop().close()
                return False
        return _S()

    def barrier(self):
        Q = self.Q["sp"]
        allsems = []
        for e in self.E.values():
            if e.cnt:
                allsems.append((e.sem, e.cnt))
        for q in self.Q.values():
            for sem, val in zip(q.sems, q.vals):
                if val:
                    allsems.append((sem, val))
        for sem, val in allsems:
            self._wait(Q.h, Q.seen, sem, val)
        self.bar_cnt += 1
        Q.h.nop().then_inc(self.bar_sem, 1)
        self.n_instr += 1
        for e in self.E.values():
            e.h.wait_ge(self.bar_sem, self.bar_cnt)
            self.n_instr += 1
            for sem, val in allsems:
                if e.seen.get(sem, 0) < val:
                    e.seen[sem] = val

    def ps(self, name, shape=(128, 512), dt=F32):
        self.uid += 1
        t = self.es.enter_context(self.nc.psum_tensor("%s_%d" % (name, self.uid), list(shape), dt))
        return Tile(t, name)

    def dram(self, name, shape, dt, kind="Internal"):
        t = self.nc.dram_tensor(name, list(shape), dt, kind=kind)
        return Tile(t.ap(), name)

    def _wait(self, h, seen, sem, val):
        if seen.get(sem, 0) >= val:
            return
        h.wait_ge(sem, val)
        self.n_instr += 1
        seen[sem] = val

    def _deps(self, h, seen, own_sem, reads, writes, disjoint):
        for b in reads:
            for sem, val in b.w.items():
                self._wait(h, seen, sem, val)
        for b in writes:
            for sem, val in b.r.items():
                if sem is own_sem:
                    continue
                self._wait(h, seen, sem, val)
            if not disjoint:
                for sem, val in b.w.items():
                    if sem is own_sem:
                        continue
                    self._wait(h, seen, sem, val)

    def _mark(self, sem, val, reads, writes, disjoint):
        for b in reads:
            if b.r.get(sem, 0) < val:
                b.r[sem] = val
        for b in writes:
            if disjoint:
                b.w[sem] = val
            else:
                b.w = {sem: val}
                b.r = {}

    def op(self, e, fn, reads=(), writes=(), disjoint=False):
        eng = self.E[e]
        self._deps(eng.h, eng.seen, eng.sem, reads, writes, disjoint)
        ins = fn(eng.h)
        eng.cnt += 1
        ins.then_inc(eng.sem, 1)
        self.n_instr += 1
        self._mark(eng.sem, eng.cnt, reads, writes, disjoint)

    def ops(self, e, fns, reads=(), writes=(), disjoint=False):
        eng = self.E[e]
        self._deps(eng.h, eng.seen, eng.sem, reads, writes, disjoint)
        ins = None
        for fn in fns:
            ins = fn(eng.h)
            self.n_instr += 1
        eng.cnt += 1
        ins.then_inc(eng.sem, 1)
        self._mark(eng.sem, eng.cnt, reads, writes, disjoint)

    def mm(self, out, pairs, reads=(), writes=()):
        n = len(pairs)
        fns = []
        for i, (l, r) in enumerate(pairs):
            fns.append(lambda h, l=l, r=r, i=i: h.matmul(out, l, r, start=(i == 0), stop=(i == n - 1)))
        self.ops("pe", fns, reads, writes)

    def dma(self, q, out, in_, reads=(), writes=(), disjoint=True, **kw):
        Q = self.Q[q]
        self._deps(Q.h, Q.seen, None, reads, writes, disjoint)
        k = Q.k
        Q.k = (k + 1) % len(Q.sems)
        sem = Q.sems[k]
        self._wait(Q.h, Q.seen, sem, Q.vals[k])
        Q.vals[k] += 16
        Q.h.dma_start(out=out, in_=in_, **kw).then_inc(sem, 16)
        self.n_instr += 1
        self._mark(sem, Q.vals[k], reads, writes, disjoint)

    def finish(self, bufs):
        Q = self.Q["sp"]
        for b in bufs:
            for sem, val in b.w.items():
                self._wait(Q.h, Q.seen, sem, val)
        for q in self.Q.values():
            for sem, val in zip(q.sems, q.vals):
                if val:
                    self._wait(Q.h, Q.seen, sem, val)

    def close(self):
        self.es.close()

import math

D = 2048
KC = 16
IN_COLS = 18528
C_QC, C_KVC, C_KPE, C_NAQ, C_NAK, C_NAV, C_LU, C_LY, C_DQKV, C_DZ, C_DA, C_DB, C_GT = (
    0, 512, 1024, 1088, 2112, 3136, 4160, 5184, 6208, 9280, 10304, 10320, 10336)
EPS = 1e-6


class Cfg:
    def __init__(self, NX=4096, NZ=256, E=32, L=4, dbg=()):
        self.NX, self.NZ, self.E, self.L = NX, NZ, E, L
        self.T = NX + NZ
        self.dbg = set(dbg)


def tok_blocks(t0, t1, bs=512):
    out = []
    t = t0
    while t < t1:
        n = min(bs, t1 - t)
        out.append((t, n))
        t += n
    return out


class Builder:
    def __init__(self, cfg):
        self.cfg = cfg
        self.nc = nc = bass.Bass("TRN2", target_bir_lowering=False)
        self.fw = fw = FW(nc)
        c = cfg
        T, L, E = c.T, c.L, c.E
        di = lambda name, shape, dt=F32: fw.dram(name, shape, dt, kind="ExternalInput")
        self.xT_in = di("xT", [D, T])
        self.cc = di("cc", [128, KC, 2])
        self.ada_w = self.bigdecl("ada_w", [L, D, 6 * D])
        self.ada_b = di("ada_b", [128, L, 96])
        self.norm_g = di("norm_g", [128, L, 2, KC])
        self.w_in = self.bigdecl("w_in", [L, D, IN_COLS])
        self.final_g = di("final_g", [128, KC])
        self.out = fw.dram("out", [D, c.NX], F32, kind="ExternalOutput")
        kind = "Internal"
        self.dbg_out = {}
        self.XT = self.scr("XT", [D, T], F32)
        self.QKVC = self.scr("QKVC", [1024, T], F32)
        self.KPE = self.scr("KPE", [128, T], F32)
        self.NAQK = self.scr("NAQK", [2048, T], BF16)
        self.NAV = self.scr("NAV", [T, 1024], BF16)
        self.LUY = self.scr("LUY", [2048, T], F32)
        self.DQKV = self.scr("DQKV", [3072, T], F32)
        self.DZ = self.scr("DZ", [1024, T], F32)
        self.GB = self.scr("GB", [T, 32], F32)
        self.SG = self.scr("SG", [8192, T], BF16)
        self.ones_bf = fw.sb("ones_bf", [128, 128], BF16)
        fw.op("pool", lambda h: h.memset(self.ones_bf[:], 1.0), writes=[self.ones_bf])
        self.ones_f = fw.sb("ones_f", [128, 128], F32)
        fw.op("pool", lambda h: h.memset(self.ones_f[:], 1.0), writes=[self.ones_f])
        self.psb = [fw.ps("ps%d" % i) for i in range(8)]
        self.psi = 0

    def bigdecl(self, name, shape):
        if getattr(self.cfg, "shard", False):
            return list(shape)
        return self.fw.dram(name, shape, F32, kind="ExternalInput")

    def scr(self, name, shape, dt):
        kind = "ExternalOutput" if name in self.cfg.dbg else "Internal"
        t = self.fw.dram(name, shape, dt, kind=kind)
        if kind == "ExternalOutput":
            self.dbg_out[name] = t
        return t

    def ps(self):
        p = self.psb[self.psi]
        self.psi = (self.psi + 1) % 8
        return p

    def build(self):
        fw, c = self.fw, self.cfg
        self.prologue()
        for l in range(c.L):
            self.layer(l)
        self.epilogue()
        outs = [self.out] + list(self.dbg_out.values())
        fw.finish(outs)
        fw.close()
        return self.nc

    def prologue(self):
        fw, c = self.fw, self.cfg
        nblk = 8
        rows = D // nblk
        for i in range(nblk):
            fw.dma("sp", self.XT[i * rows:(i + 1) * rows, :], self.xT_in[i * rows:(i + 1) * rows, :],
                   reads=[self.xT_in], writes=[self.XT])
        self.cc_sb = fw.sb("cc_sb", [128, KC, 2], F32)
        fw.dma("sp", self.cc_sb[:], self.cc[:], writes=[self.cc_sb])
        self.scc = fw.sb("scc", [128, KC, 2], F32)
        fw.op("act", lambda h: h.activation(out=self.scc[:], in_=self.cc_sb[:], func=AF.Silu),
              reads=[self.cc_sb], writes=[self.scc])
        self.ada_b_sb = fw.sb("ada_b_sb", [128, c.L, 96], F32)
        fw.dma("sp", self.ada_b_sb[:], self.ada_b[:], writes=[self.ada_b_sb])
        self.norm_g_sb = fw.sb("norm_g_sb", [128, c.L, 2, KC], F32)
        fw.dma("sp", self.norm_g_sb[:], self.norm_g[:], writes=[self.norm_g_sb])
        self.mod = fw.sb("mod", [128, 96, 2], F32)
        self.gs = fw.sb("gs", [128, 2, KC, 2], F32)
        self.eps_t = fw.sb("eps_t", [128, 1], F32)
        fw.op("pool", lambda h: h.memset(self.eps_t[:], EPS), writes=[self.eps_t])

    def adaln(self, l):
        fw = self.fw
        with fw.scope():
            self.adaw = [fw.sb("adaw%d" % i, [128, KC, 512], F32) for i in range(2)]
            self._adaln(l)

    def _adaln(self, l):
        fw = self.fw
        for jb in range(24):
            wt = self.adaw[jb % 2]
            fw.dma("sp", wt[:], self.ada_w[l, :, jb * 512:(jb + 1) * 512].rearrange("(k p) n -> p k n", p=128),
                   reads=[self.ada_w], writes=[wt])
            ps = self.ps()
            for m in range(4):
                fw.mm(ps[:, m * 2:(m + 1) * 2],
                      [(wt[:, k, m * 128:(m + 1) * 128], self.scc[:, k, :]) for k in range(KC)],
                      reads=[wt, self.scc], writes=[ps])
            fw.op("dve", lambda h, ps=ps, jb=jb: h.tensor_tensor(
                out=self.mod[:, jb * 4:(jb + 1) * 4, :],
                in0=ps[:, 0:8].rearrange("p (m s) -> p m s", s=2),
                in1=self.ada_b_sb[:, l, jb * 4:(jb + 1) * 4].unsqueeze(2).to_broadcast([128, 4, 2]),
                op=ALU.add), reads=[ps, self.ada_b_sb], writes=[self.mod], disjoint=True)
        for n, j in ((0, 1), (1, 4)):
            fw.op("dve", lambda h, n=n, j=j: h.scalar_tensor_tensor(
                out=self.gs[:, n, :, :], in0=self.mod[:, j * 16:(j + 1) * 16, :], scalar=1.0,
                in1=self.norm_g_sb[:, l, n, :].unsqueeze(2).to_broadcast([128, KC, 2]),
                op0=ALU.add, op1=ALU.mult), reads=[self.mod, self.norm_g_sb], writes=[self.gs], disjoint=True)

    def norm_block(self, src, t0, n, s, nidx, shift_j, hT, hcol, xt, h32=None):
        fw = self.fw
        fw.dma("sp", xt[:, :, 0:n], src[:, t0:t0 + n].rearrange("(k p) t -> p k t", p=128),
               reads=[src], writes=[xt])
        sq = self.sqt
        fw.op("act", lambda h: h.activation(out=sq[:, :, 0:n], in_=xt[:, :, 0:n], func=AF.Square),
              reads=[xt], writes=[sq])
        ps = self.ps()
        fw.mm(ps[:, 0:n], [(self.ones_bf[:], sq[:, k, 0:n]) for k in range(KC)], reads=[sq, self.ones_bf], writes=[ps])
        rs = self.rst
        fw.op("act", lambda h: h.activation(out=rs[:, 0:n], in_=ps[:, 0:n], func=AF.Sqrt, scale=1.0 / D, bias=self.eps_t[:]),
              reads=[ps, self.eps_t], writes=[rs])
        fw.op("dve", lambda h: h.reciprocal(out=rs[:, 0:n], in_=rs[:, 0:n]), reads=[rs], writes=[rs])
        fw.op("dve", lambda h: h.tensor_tensor(out=xt[:, :, 0:n], in0=xt[:, :, 0:n],
                                                in1=rs[:, 0:n].unsqueeze(1).to_broadcast([128, KC, n]), op=ALU.mult),
              reads=[xt, rs], writes=[xt])
        for k in range(KC):
            eng = "act" if k % 2 == 0 else "pool"
            if eng == "act":
                fw.op("act", lambda h, k=k: h.activation(
                    out=hT[:, k, hcol:hcol + n], in_=xt[:, k, 0:n], func=AF.Identity,
                    scale=self.gs[:, nidx, k, s:s + 1], bias=self.mod[:, shift_j * 16 + k, s:s + 1]),
                    reads=[xt, self.gs, self.mod], writes=[hT], disjoint=True)
            else:
                fw.op("pool", lambda h, k=k: h.tensor_scalar(
                    out=hT[:, k, hcol:hcol + n], in0=xt[:, k, 0:n],
                    scalar1=self.gs[:, nidx, k, s:s + 1], scalar2=self.mod[:, shift_j * 16 + k, s:s + 1],
                    op0=ALU.mult, op1=ALU.add),
                    reads=[xt, self.gs, self.mod], writes=[hT], disjoint=True)
            if h32 is not None:
                fw.op("dve", lambda h, k=k: h.tensor_scalar(
                    out=h32[:, k, 0:n], in0=xt[:, k, 0:n],
                    scalar1=self.gs[:, nidx, k, s:s + 1], scalar2=self.mod[:, shift_j * 16 + k, s:s + 1],
                    op0=ALU.mult, op1=ALU.add),
                    reads=[xt, self.gs, self.mod], writes=[h32], disjoint=True)

    def halves(self):
        c = self.cfg
        hx = c.NX // 2
        return [(0, hx), (hx, c.T)]

    def layer(self, l):
        self.adaln(l)
        self.proj_in(l)

    def proj_in(self, l):
        fw, c = self.fw, self.cfg
        with fw.scope():
            hmax = max(b - a for a, b in self.halves())
            self.hT = fw.sb("hT", [128, KC, hmax], BF16)
            self.xts = [fw.sb("xt%d" % i, [128, KC, 256], F32) for i in range(2)]
            self.sqt = fw.sb("sqt", [128, KC, 256], BF16)
            self.rst = fw.sb("rst", [128, 512], F32)
            self.wbuf = [fw.sb("wbuf%d" % i, [128, KC, 512], BF16) for i in range(2)]
            self.stg = [fw.sb("stg%d" % i, [128, 4, 512], F32) for i in range(2)]
            self.stgb = [fw.sb("stgb%d" % i, [128, 4, 512], BF16) for i in range(2)]
            self.wi = 0
            self.si = 0
            self._proj_in(l)

    def _proj_in(self, l):
        fw, c = self.fw, self.cfg
        hT = self.hT
        for (h0, h1) in self.halves():
            i = 0
            for (t0, n) in tok_blocks(h0, h1, 256):
                s = 0 if t0 < c.NX else 1
                self.norm_block(self.XT, t0, n, s, 0, 0, hT, t0 - h0, self.xts[i % 2])
                i += 1
            nt = h1 - h0
            tb = tok_blocks(0, nt)
            fm_groups = [
                (C_QC, 1024, self.QKVC, 0, F32, None),
                (C_NAQ, 2048, self.NAQK, 0, BF16, None),
                (C_LU, 2048, self.LUY, 0, F32, None),
                (C_DQKV, 3072, self.DQKV, 0, F32, None),
                (C_DZ, 1024, self.DZ, 0, F32, None),
                (C_GT, 8192, self.SG, 0, BF16, AF.Sigmoid),
            ]
            for (c0, ncols, dst, r0, dt, func) in fm_groups:
                for j in range(ncols // 512):
                    wt = self.load_w(self.w_in[l, :, c0 + j * 512:c0 + (j + 1) * 512])
                    for (t0, n) in tb:
                        st = (self.stg if dt == F32 else self.stgb)[self.si % 2]
                        self.si += 1
                        for m in range(4):
                            ps = self.ps()
                            fw.mm(ps[:, 0:n], [(wt[:, k, m * 128:(m + 1) * 128], hT[:, k, t0:t0 + n]) for k in range(KC)],
                                  reads=[wt, hT], writes=[ps])
                            self.evac(ps[:, 0:n], st[:, m, 0:n], ps, st, func, m)
                        fw.dma("sp", dst[r0 + j * 512:r0 + (j + 1) * 512, h0 + t0:h0 + t0 + n].rearrange("(m p) t -> p m t", p=128),
                               st[:, :, 0:n], reads=[st], writes=[dst])
            wt = self.wbuf[self.wi % 2]
            self.wi += 1
            srcs = [(0, 64, C_KPE)]
            for a in range(2):
                srcs.append((64 + a * 32, 16, C_KPE + a * 32 + 16))
                srcs.append((64 + a * 32 + 16, 16, C_KPE + a * 32))
            for (o, w, cc0) in srcs:
                fw.dma("pool", wt[:, :, o:o + w], self.w_in[l, :, cc0:cc0 + w].rearrange("(k p) n -> p k n", p=128),
                       reads=[self.w_in], writes=[wt])
            for (t0, n) in tb:
                st = self.stg[self.si % 2]
                self.si += 1
                ps = self.ps()
                fw.mm(ps[:, 0:n], [(wt[:, k, 0:128], hT[:, k, t0:t0 + n]) for k in range(KC)], reads=[wt, hT], writes=[ps])
                self.evac(ps[:, 0:n], st[:, 0, 0:n], ps, st, None, 0)
                fw.dma("sp", self.KPE[:, h0 + t0:h0 + t0 + n], st[:, 0, 0:n], reads=[st], writes=[self.KPE])
            for j in range(2):
                wt = self.load_w(self.w_in[l, :, C_NAV + j * 512:C_NAV + (j + 1) * 512])
                for tt in range(nt // 128):
                    st = self.stgb[self.si % 2]
                    self.si += 1
                    ps = self.ps()
                    fw.mm(ps[:, :], [(hT[:, k, tt * 128:(tt + 1) * 128], wt[:, k, :]) for k in range(KC)], reads=[wt, hT], writes=[ps])
                    self.evac(ps[:, :], st[:, 0, :], ps, st, None, tt)
                    fw.dma("sp", self.NAV[h0 + tt * 128:h0 + (tt + 1) * 128, j * 512:(j + 1) * 512], st[:, 0, :],
                           reads=[st], writes=[self.NAV])
            wt = self.wbuf[self.wi % 2]
            self.wi += 1
            fw.dma("pool", wt[:, :, 0:32], self.w_in[l, :, C_DA:C_DA + 32].rearrange("(k p) n -> p k n", p=128),
                   reads=[self.w_in], writes=[wt])
            for tt in range(nt // 128):
                st = self.stg[self.si % 2]
                self.si += 1
                ps = self.ps()
                fw.mm(ps[:, 0:32], [(hT[:, k, tt * 128:(tt + 1) * 128], wt[:, k, 0:32]) for k in range(KC)], reads=[wt, hT], writes=[ps])
                self.evac(ps[:, 0:32], st[:, 0, 0:32], ps, st, None, tt)
                fw.dma("sp", self.GB[h0 + tt * 128:h0 + (tt + 1) * 128, :], st[:, 0, 0:32], reads=[st], writes=[self.GB])

    def load_w(self, src, q="pool"):
        wt = self.wbuf[self.wi % 2]
        self.wi += 1
        self.fw.dma(q, wt[:], src.rearrange("(k p) n -> p k n", p=128), reads=[], writes=[wt])
        return wt

    def evac(self, src, dst, psb, stb, func, parity):
        fw = self.fw
        if func is not None:
            fw.op("act", lambda h: h.activation(out=dst, in_=src, func=func), reads=[psb], writes=[stb], disjoint=True)
        elif parity % 2 == 0:
            fw.op("act", lambda h: h.copy(out=dst, in_=src), reads=[psb], writes=[stb], disjoint=True)
        else:
            fw.op("dve", lambda h: h.tensor_copy(out=dst, in_=src), reads=[psb], writes=[stb], disjoint=True)

    def epilogue(self):
        fw, c = self.fw, self.cfg
        nblk = 8
        rows = D // nblk
        for i in range(nblk):
            fw.dma("sp", self.out[i * rows:(i + 1) * rows, :], self.XT[i * rows:(i + 1) * rows, 0:c.NX],
                   reads=[self.XT], writes=[self.out])


class Builder2(Builder):
    def __init__(self, cfg):
        Builder.__init__(self, cfg)
        fw, c = self.fw, cfg
        L, E, T = c.L, c.E, c.T
        di = lambda name, shape, dt=F32: fw.dram(name, shape, dt, kind="ExternalInput")
        self.w_branch = self.bigdecl("w_branch", [L, 4, 1024, D])
        self.w_out = self.bigdecl("w_out", [L, D, D])
        self.router_w = di("router_w", [L, D, E])
        self.router_b = di("router_b", [1, L, E])
        self.exp_w1 = self.bigdecl("exp_w1", [L, E, D, 1280])
        self.exp_b1 = di("exp_b1", [128, L, E, 2, 5])
        self.exp_w2 = self.bigdecl("exp_w2", [L, E, 640, D])
        self.exp_b2 = di("exp_b2", [L, E, D])
        self.ident_in = di("ident", [128, 128])
        self.OBR = self.scr("OBR", [4096, T], BF16)
        self.ident = fw.sb("ident", [128, 128], F32)
        fw.dma("sp", self.ident[:], self.ident_in[:], writes=[self.ident])
        self.psA, self.psB = self.psb[6], self.psb[7]

    def ps(self):
        p = self.psb[self.psi]
        self.psi = (self.psi + 1) % 6
        return p

    def layer(self, l):
        self.adaln(l)
        self.proj_in(l)
        self.mixers(l)
        self.merge(l)
        self.moe(l)

    def mixers(self, l):
        pass

    def merge(self, l):
        fw, c = self.fw, self.cfg
        last = (l == c.L - 1)
        t_end = c.NX if last else c.T
        with fw.scope():
            ob = [fw.sb("ob%d" % i, [128, 8, 512], BF16) for i in range(2)]
            sg = [fw.sb("sg%d" % i, [128, 4, 512], BF16) for i in range(2)]
            wb = [fw.sb("wbb%d" % i, [128, 8, 512], BF16) for i in range(2)]
            wo = [fw.sb("wo%d" % i, [128, KC, 512], BF16) for i in range(2)]
            macc = fw.sb("macc", [128, KC, 512], F32)
            mbf = fw.sb("mbf", [128, KC, 512], BF16)
            tmp = [fw.sb("mtmp%d" % i, [128, 512], F32) for i in range(2)]
            xt = [fw.sb("mxt%d" % i, [128, 4, 512], F32) for i in range(2)]
            cnt = 0
            for (t0, n) in tok_blocks(0, t_end):
                s = 0 if t0 < c.NX else 1
                for i in range(4):
                    o = ob[i % 2]
                    fw.dma("sp", o[:, :, 0:n], self.OBR[i * 1024:(i + 1) * 1024, t0:t0 + n].rearrange("(k p) t -> p k t", p=128),
                           reads=[self.OBR], writes=[o])
                    for mb in range(4):
                        w = wb[cnt % 2]
                        g = sg[cnt % 2]
                        cnt += 1
                        fw.dma("pool", w[:], self.w_branch[l, i, :, mb * 512:(mb + 1) * 512].rearrange("(k p) n -> p k n", p=128),
                               writes=[w])
                        fw.dma("sp", g[:, :, 0:n], self.SG[i * D + mb * 512:i * D + (mb + 1) * 512, t0:t0 + n].rearrange("(m p) t -> p m t", p=128),
                               reads=[self.SG], writes=[g])
                        for m in range(4):
                            mm_ = mb * 4 + m
                            ps = self.ps()
                            fw.mm(ps[:, 0:n], [(w[:, k, m * 128:(m + 1) * 128], o[:, k, 0:n]) for k in range(8)],
                                  reads=[w, o], writes=[ps])
                            if i == 0:
                                fw.op("dve", lambda h, ps=ps, g=g, m=m, mm_=mm_: h.tensor_tensor(
                                    out=macc[:, mm_, 0:n], in0=ps[:, 0:n], in1=g[:, m, 0:n], op=ALU.mult),
                                    reads=[ps, g], writes=[macc], disjoint=True)
                            else:
                                tp = tmp[mm_ % 2]
                                fw.op("dve", lambda h, ps=ps, g=g, m=m, tp=tp: h.tensor_tensor(
                                    out=tp[:, 0:n], in0=ps[:, 0:n], in1=g[:, m, 0:n], op=ALU.mult),
                                    reads=[ps, g], writes=[tp])
                                if i < 3:
                                    fw.op("pool", lambda h, tp=tp, mm_=mm_: h.tensor_tensor(
                                        out=macc[:, mm_, 0:n], in0=macc[:, mm_, 0:n], in1=tp[:, 0:n], op=ALU.add),
                                        reads=[tp, macc], writes=[macc], disjoint=True)
                                else:
                                    fw.op("pool", lambda h, tp=tp, mm_=mm_: h.tensor_tensor(
                                        out=mbf[:, mm_, 0:n], in0=macc[:, mm_, 0:n], in1=tp[:, 0:n], op=ALU.add),
                                        reads=[tp, macc], writes=[mbf], disjoint=True)
                for mb in range(4):
                    w = wo[mb % 2]
                    fw.dma("pool", w[:], self.w_out[l, :, mb * 512:(mb + 1) * 512].rearrange("(k p) n -> p k n", p=128), writes=[w])
                    x = xt[mb % 2]
                    fw.dma("sp", x[:, :, 0:n], self.XT[mb * 512:(mb + 1) * 512, t0:t0 + n].rearrange("(m p) t -> p m t", p=128),
                           reads=[self.XT], writes=[x])
                    for m in range(4):
                        mm_ = mb * 4 + m
                        ps = self.ps()
                        fw.mm(ps[:, 0:n], [(w[:, k, m * 128:(m + 1) * 128], mbf[:, k, 0:n]) for k in range(KC)],
                              reads=[w, mbf], writes=[ps])
                        tp = tmp[mm_ % 2]
                        fw.op("act", lambda h, ps=ps, tp=tp, mm_=mm_: h.activation(
                            out=tp[:, 0:n], in_=ps[:, 0:n], func=AF.Identity, scale=self.mod[:, 2 * 16 + mm_, s:s + 1]),
                            reads=[ps, self.mod], writes=[tp])
                        fw.op("dve", lambda h, tp=tp, x=x, m=m: h.tensor_tensor(
                            out=x[:, m, 0:n], in0=x[:, m, 0:n], in1=tp[:, 0:n], op=ALU.add), reads=[tp, x], writes=[x], disjoint=True)
                    fw.dma("sp", self.XT[mb * 512:(mb + 1) * 512, t0:t0 + n].rearrange("(m p) t -> p m t", p=128), x[:, :, 0:n],
                           reads=[x], writes=[self.XT])

    def moe_blocks(self, t_end):
        out = []
        t = 0
        while t < t_end:
            n = min(1152, t_end - t)
            out.append((t, n))
            t += n
        return out

    def moe(self, l):
        fw, c = self.fw, self.cfg
        E = c.E
        last = (l == c.L - 1)
        t_end = c.NX if last else c.T
        TBMAX = min(1152, t_end)
        with fw.scope():
            hT = fw.sb("h2T", [128, KC, TBMAX], BF16)
            acc = fw.sb("acc", [128, KC, TBMAX], F32)
            w1u = [fw.sb("w1u%d" % i, [128, KC, 256], BF16) for i in range(2)]
            w2u = [fw.sb("w2u%d" % i, [128, 5, 512], BF16) for i in range(2)]
            apt = fw.sb("apt", [128, 5, TBMAX], BF16)
            b1 = fw.sb("b1", [128, E, 2, 5], F32)
            fw.dma("sp", b1[:], self.exp_b1[:, l], writes=[b1])
            rw = fw.sb("rw", [128, KC, E], F32)
            fw.dma("sp", rw[:], self.router_w[l].rearrange("(k p) e -> p k e", p=128), writes=[rw])
            rb = fw.sb("rb", [1, E], F32)
            fw.dma("sp", rb[:], self.router_b[:, l, :], writes=[rb])
            xt = [fw.sb("nxt%d" % i, [128, KC, 128], F32) for i in range(1)] * 2
            h32 = acc
            self.sqt = fw.sb("sqt2", [128, KC, 128], BF16)
            self.rst = fw.sb("rst2", [128, 512], F32)
            lg = fw.sb("lg", [128, E], F32)
            m8 = fw.sb("m8", [128, 8], F32)
            nm = fw.sb("nm", [128, 1], F32)
            ssum = fw.sb("ssum", [128, 1], F32)
            msk = fw.sb("msk", [128, E], F32)
            ex32 = fw.sb("ex", [128, 32], F32)
            fw.op("pool", lambda h: h.memset(ex32[:], 0.0), writes=[ex32])

            comb_tm = fw.sb("comb_tm", [128, TBMAX // 128, E], F32)
            cbe = [fw.sb("cbe%d" % i, [128, 128], BF16) for i in range(2)]
            cbs = fw.sb("cbs", [128, TBMAX], BF16)
            b2pad = [fw.sb("b2pad%d" % i, [128, D], BF16) for i in range(2)]
            for t_ in b2pad:
                fw.op("pool", lambda h, t_=t_: h.memset(t_[:], 0.0), writes=[t_])
            exb = fw.sb("exb", [128, 32], BF16)
            identb = fw.sb("identb", [128, 128], BF16)
            fw.op("dve", lambda h: h.tensor_copy(out=identb[:], in_=self.ident[:]), reads=[self.ident], writes=[identb])

            class _V:
                pass
            ex = Tile(ex32.t, "exv")
            ex.w, ex.r = ex32.w, ex32.r
            _exview = ex32
            ytmp = [fw.sb("ytmp%d" % i, [128, 512], F32) for i in range(2)]
            g_ = fw.sb("gt", [128, 512], F32)
            s_ = fw.sb("sgt", [128, 512], F32)
            l_ = fw.sb("lt", [128, 512], F32)
            wi = 0
            w2i = 0
            for (b0, bn) in self.moe_blocks(t_end):
                for tt in range(bn // 128):
                    t0 = b0 + tt * 128
                    s = 0 if t0 < c.NX else 1
                    self.norm_block(self.XT, t0, 128, s, 1, 3, hT, tt * 128, xt[tt % 2], h32=h32)
                    MS = getattr(c, "moe_stage", 9)
                    if MS < 0.2: continue
                    ps = self.ps()
                    pairs = [(h32[:, k, 0:128], rw[:, k, :]) for k in range(KC)] + [(self.ones_f[0:1, :], rb[:])]
                    fw.mm(ps[:, 0:E], pairs, reads=[h32, rw, rb, self.ones_f], writes=[ps])
                    fw.op("dve", lambda h, ps=ps: h.tensor_copy(out=lg[:], in_=ps[:, 0:E]), reads=[ps], writes=[lg])
                    if MS < 0.3: continue
                    fw.op("dve", lambda h: h.max(out=m8[:], in_=lg[:]), reads=[lg], writes=[m8])
                    fw.op("dve", lambda h: h.tensor_scalar(out=msk[:], in0=lg[:], scalar1=m8[:, 3:4], scalar2=None, op0=ALU.is_ge),
                          reads=[lg, m8], writes=[msk])
                    fw.op("dve", lambda h: h.tensor_scalar(out=nm[:], in0=m8[:, 0:1], scalar1=-1.0, scalar2=None, op0=ALU.mult),
                          reads=[m8], writes=[nm])
                    fw.op("act", lambda h: h.activation(out=ex32[:, 0:E], in_=lg[:], func=AF.Exp, bias=nm[:], scale=1.0),
                          reads=[lg, nm], writes=[ex32])
                    fw.op("dve", lambda h: h.tensor_tensor(out=ex32[:, 0:E], in0=ex32[:, 0:E], in1=msk[:], op=ALU.mult), reads=[ex32, msk], writes=[ex32])
                    fw.op("dve", lambda h: h.reduce_sum(out=ssum[:], in_=ex32[:, 0:E], axis=AX.X), reads=[ex32], writes=[ssum])
                    fw.op("dve", lambda h: h.reciprocal(out=ssum[:], in_=ssum[:]), reads=[ssum], writes=[ssum])
                    fw.op("dve", lambda h: h.tensor_scalar(out=ex32[:, 0:E], in0=ex32[:, 0:E], scalar1=ssum[:, 0:1], scalar2=None, op0=ALU.mult),
                          reads=[ex32, ssum], writes=[ex32])
                    fw.op("dve", lambda h, tt=tt: h.tensor_copy(out=comb_tm[:, tt, :], in_=ex32[:, 0:E]), reads=[ex32], writes=[comb_tm], disjoint=True)
                subs = tok_blocks(0, bn)
                MS = getattr(c, "moe_stage", 9)
                if MS < 1: continue
                fw.op("pool", lambda h: h.memset(acc[:], 0.0), writes=[acc])
                for e in (range(E) if MS >= 2 else []):
                    b2p = b2pad[e % 2]
                    fw.dma("pool", b2p[0:1, :], self.exp_b2[l, e:e + 1, :], writes=[b2p])
                    for (t0, n) in subs:
                        psc = self.ps()
                        for q in range(n // 128):
                            tt = t0 // 128 + q
                            cb_ = cbe[tt % 2]
                            fw.op("dve", lambda h, cb_=cb_, tt=tt: h.tensor_copy(out=cb_[:], in_=comb_tm[:, tt, e:e + 1].to_broadcast([128, 128])),
                                  reads=[comb_tm], writes=[cb_])
                            fw.mm(psc[:, q * 128:(q + 1) * 128], [(cb_[:], identb[:])], reads=[cb_, identb], writes=[psc])
                        fw.op("act", lambda h, psc=psc: h.copy(out=cbs[:, t0:t0 + n], in_=psc[:, 0:n]), reads=[psc], writes=[cbs], disjoint=True)
                    for j in range(5):
                        w1 = w1u[wi % 2]
                        wi += 1
                        for hf in range(2):
                            fw.dma("pool", w1[:, hf * 8:(hf + 1) * 8, :],
                                   self.exp_w1[l, e, hf * 1024:(hf + 1) * 1024, j * 256:(j + 1) * 256].rearrange("(k p) n -> p k n", p=128),
                                   writes=[w1])
                        for (t0, n) in subs:
                            psg = self.ps()
                            fw.mm(psg[:, 0:n], [(w1[:, k, 0:256:2], hT[:, k, t0:t0 + n]) for k in range(KC)],
                                  reads=[w1, hT], writes=[psg])
                            psl = self.ps()
                            fw.mm(psl[:, 0:n], [(w1[:, k, 1:256:2], hT[:, k, t0:t0 + n]) for k in range(KC)],
                                  reads=[w1, hT], writes=[psl])
                            fw.op("dve", lambda h, psg=psg, j=j: h.tensor_scalar(
                                out=g_[:, 0:n], in0=psg[:, 0:n], scalar1=b1[:, e, 0, j:j + 1], scalar2=7.0, op0=ALU.add, op1=ALU.min),
                                reads=[psg, b1], writes=[g_])
                            fw.op("act", lambda h: h.activation(out=s_[:, 0:n], in_=g_[:, 0:n], func=AF.Sigmoid, scale=1.702),
                                  reads=[g_], writes=[s_])
                            fw.op("dve", lambda h, psl=psl, j=j: h.tensor_scalar(
                                out=l_[:, 0:n], in0=psl[:, 0:n], scalar1=b1[:, e, 1, j:j + 1], scalar2=7.0, op0=ALU.add, op1=ALU.min),
                                reads=[psl, b1], writes=[l_])
                            fw.op("dve", lambda h: h.tensor_scalar(
                                out=l_[:, 0:n], in0=l_[:, 0:n], scalar1=-7.0, scalar2=1.0, op0=ALU.max, op1=ALU.add),
                                reads=[l_], writes=[l_])
                            fw.op("pool", lambda h: h.tensor_tensor(out=g_[:, 0:n], in0=g_[:, 0:n], in1=s_[:, 0:n], op=ALU.mult),
                                  reads=[g_, s_], writes=[g_])
                            fw.op("dve", lambda h: h.tensor_tensor(out=s_[:, 0:n], in0=l_[:, 0:n], in1=cbs[:, t0:t0 + n], op=ALU.mult),
                                  reads=[l_, cbs, g_], writes=[s_])
                            fw.op("dve", lambda h, j=j: h.tensor_tensor(out=apt[:, j, t0:t0 + n], in0=g_[:, 0:n], in1=s_[:, 0:n], op=ALU.mult),
                                  reads=[g_, s_], writes=[apt], disjoint=True)
                    for mb in (range(4) if MS >= 3 else []):
                      w2 = w2u[w2i % 2]
                      w2i += 1
                      fw.dma("pool", w2[:], self.exp_w2[l, e, :, mb * 512:(mb + 1) * 512].rearrange("(k p) n -> p k n", p=128), writes=[w2])
                      for (t0, n) in subs:
                        for m in range(mb * 4, mb * 4 + 4):
                            ps = self.ps()
                            fw.mm(ps[:, 0:n], [(w2[:, j, (m % 4) * 128:(m % 4 + 1) * 128], apt[:, j, t0:t0 + n]) for j in range(5)]
                                  + [(b2p[:, m * 128:(m + 1) * 128], cbs[:, t0:t0 + n])],
                                  reads=[w2, apt, b2p, cbs], writes=[ps])
                            tp = ytmp[m % 2]
                            fw.op("act", lambda h, ps=ps, tp=tp: h.copy(out=tp[:, 0:n], in_=ps[:, 0:n]), reads=[ps], writes=[tp])
                            fw.op("dve" if m % 2 else "pool", lambda h, tp=tp, m=m: h.tensor_tensor(out=acc[:, m, t0:t0 + n], in0=acc[:, m, t0:t0 + n], in1=tp[:, 0:n], op=ALU.add),
                                  reads=[tp, acc], writes=[acc], disjoint=True)
                for tt in (range(bn // 128) if MS >= 4 else []):
                    t0 = b0 + tt * 128
                    s = 0 if t0 < c.NX else 1
                    x = xt[tt % 2]
                    fw.dma("sp", x[:], self.XT[:, t0:t0 + 128].rearrange("(k p) t -> p k t", p=128), reads=[self.XT], writes=[x])
                    for m in range(KC):
                        fw.op("dve" if m % 2 else "pool", lambda h, x=x, m=m: h.scalar_tensor_tensor(
                            out=x[:, m, :], in0=acc[:, m, tt * 128:(tt + 1) * 128], scalar=self.mod[:, 5 * 16 + m, s:s + 1], in1=x[:, m, :],
                            op0=ALU.mult, op1=ALU.add), reads=[acc, x, self.mod], writes=[x], disjoint=True) if m % 2 else \
                            fw.op("dve", lambda h, x=x, m=m: h.scalar_tensor_tensor(
                                out=x[:, m, :], in0=acc[:, m, tt * 128:(tt + 1) * 128], scalar=self.mod[:, 5 * 16 + m, s:s + 1], in1=x[:, m, :],
                                op0=ALU.mult, op1=ALU.add), reads=[acc, x, self.mod], writes=[x], disjoint=True)
                    fw.dma("sp", self.XT[:, t0:t0 + 128].rearrange("(k p) t -> p k t", p=128), x[:], reads=[x], writes=[self.XT])

    def epilogue(self):
        fw, c = self.fw, self.cfg
        with fw.scope():
            fg = fw.sb("fg", [128, KC], F32)
            fw.dma("sp", fg[:], self.final_g[:], writes=[fg])
            xt = [fw.sb("fxt%d" % i, [128, KC, 256], F32) for i in range(2)]
            sq = fw.sb("fsq", [128, KC, 256], BF16)
            rs = fw.sb("frs", [128, 256], F32)
            i = 0
            for (t0, n) in tok_blocks(0, c.NX, 256):
                x = xt[i % 2]
                i += 1
                fw.dma("sp", x[:, :, 0:n], self.XT[:, t0:t0 + n].rearrange("(k p) t -> p k t", p=128), reads=[self.XT], writes=[x])
                fw.op("act", lambda h, x=x: h.activation(out=sq[:, :, 0:n], in_=x[:, :, 0:n], func=AF.Square), reads=[x], writes=[sq])
                ps = self.ps()
                fw.mm(ps[:, 0:n], [(self.ones_bf[:], sq[:, k, 0:n]) for k in range(KC)], reads=[sq, self.ones_bf], writes=[ps])
                fw.op("act", lambda h, ps=ps: h.activation(out=rs[:, 0:n], in_=ps[:, 0:n], func=AF.Sqrt, scale=1.0 / D, bias=self.eps_t[:]),
                      reads=[ps, self.eps_t], writes=[rs])
                fw.op("dve", lambda h: h.reciprocal(out=rs[:, 0:n], in_=rs[:, 0:n]), reads=[rs], writes=[rs])
                fw.op("dve", lambda h, x=x: h.tensor_tensor(out=x[:, :, 0:n], in0=x[:, :, 0:n],
                                                             in1=rs[:, 0:n].unsqueeze(1).to_broadcast([128, KC, n]), op=ALU.mult),
                      reads=[x, rs], writes=[x])
                fw.op("pool", lambda h, x=x: h.tensor_tensor(out=x[:, :, 0:n], in0=x[:, :, 0:n],
                                                              in1=fg[:].unsqueeze(2).to_broadcast([128, KC, n]), op=ALU.mult),
                      reads=[x, fg], writes=[x])
                fw.dma("sp", self.out[:, t0:t0 + n].rearrange("(k p) t -> p k t", p=128), x[:, :, 0:n], reads=[x], writes=[self.out])


class Builder3(Builder2):
    def __init__(self, cfg):
        Builder2.__init__(self, cfg)
        fw, c = self.fw, cfg
        L, E, T, NX = c.L, c.E, c.T, c.NX
        di = lambda name, shape, dt=F32: fw.dram(name, shape, dt, kind="ExternalInput")
        self.mla_g = di("mla_g", [128, L, 8])
        self.wq_up = di("mla_wq_up", [L, 512, 1536])
        self.wkv_up = di("mla_wkv_up", [L, 512, 2048])
        self.rope_cos = di("rope_cos", [64, NX])
        self.rope_sin = di("rope_sin", [64, NX])
        self.na_rpb = di("na_rpb", [L, 8, 15 * 31])
        self.na_mask = di("na_mask", [NX // 512, 8, 128, 512], BF16)
        self.lru_cw = di("lru_cw", [128, L, 8, 4])
        self.lru_cb = di("lru_cb", [128, L, 8])
        self.lru_wa = di("lru_wa", [L, 2, 8, 128, 128])
        self.lru_wx = di("lru_wx", [L, 2, 8, 128, 128])
        self.lru_ba = di("lru_ba", [128, L, 2, 8])
        self.lru_bx = di("lru_bx", [128, L, 2, 8])
        self.lru_lam = di("lru_lam", [128, L, 2, 8])
        self.QN = self.scr("QN", [8, 128, T], BF16)
        self.QP = self.scr("QP", [8, 64, T], BF16)
        self.KN = self.scr("KN", [8, 128, T], BF16)
        self.VT = self.scr("VT", [T, 1024], BF16)
        self.RT = self.scr("RT", [8, 23 * 127], F32)

    def mixers(self, l):
        self.mla(l)
        self.nattn(l)
        self.lru(l)

    def attn_core(self, qparts, kparts, vt, kts, nq, scale, dst, pts, extra=None):
        fw = self.fw
        pso, psl = self.psA, self.psB
        nk = len(kts)
        for i, kt in enumerate(kts):
            ps = self.ps()
            fw.mm(ps[:, 0:nq], [(kp[0][kp[1]:kp[2], kt * 128:(kt + 1) * 128], qp) for kp, qp in zip(kparts, qparts)],
                  reads=[kp[0] for kp in kparts] + [self.qtile], writes=[ps])
            pt = pts[i % len(pts)]
            fw.op("act", lambda h, ps=ps, pt=pt: h.activation(out=pt[:, 0:nq], in_=ps[:, 0:nq], func=AF.Exp, scale=scale),
                  reads=[ps], writes=[pt])
            if extra is not None:
                extra(kt, pt)
            fw.ops("pe", [lambda h, pt=pt, kt=kt, i=i: h.matmul(pso[:, 0:nq], vt[:, kt, :], pt[:, 0:nq], start=(i == 0), stop=(i == nk - 1))],
                   reads=[vt, pt], writes=[pso])
            fw.ops("pe", [lambda h, pt=pt, i=i: h.matmul(psl[:, 0:nq], self.ones_bf[:], pt[:, 0:nq], start=(i == 0), stop=(i == nk - 1))],
                   reads=[pt, self.ones_bf], writes=[psl])
        rinv = self.rinv
        fw.op("dve", lambda h: h.reciprocal(out=rinv[:, 0:nq], in_=psl[:, 0:nq]), reads=[psl], writes=[rinv])
        ot = self.otile[self.oti % 2]
        self.oti += 1
        fw.op("dve", lambda h, ot=ot: h.tensor_tensor(out=ot[:, 0:nq], in0=pso[:, 0:nq], in1=rinv[:, 0:nq], op=ALU.mult),
              reads=[pso, rinv], writes=[ot])
        fw.dma("sp", dst, ot[:, 0:nq], reads=[ot], writes=[self.OBR])

    def attn_common_tiles(self):
        fw = self.fw
        self.rinv = fw.sb("rinv", [128, 512], F32)
        self.otile = [fw.sb("otile%d" % i, [128, 512], BF16) for i in range(2)]
        self.oti = 0
        self.pts = [fw.sb("pt%d" % i, [128, 512], BF16) for i in range(3)]

    def mla(self, l):
        fw, c = self.fw, self.cfg
        T, NX = c.T, c.NX
        last = (l == c.L - 1)
        NT = T // 128
        with fw.scope():
          kp = fw.sb("kp", [64, T], BF16)
          with fw.scope():
            qkvn = fw.sb("qkvn", [128, 8, T], BF16)
            g = fw.sb("mlag", [128, 8], F32)
            fw.dma("sp", g[:], self.mla_g[:, l, :], writes=[g])
            xt = [fw.sb("mxt%d" % i, [128, 8, 512], F32) for i in range(1)] * 2
            sq = fw.sb("msq", [128, 8, 512], BF16)
            rs = fw.sb("mrs", [128, 512], F32)
            bi = 0
            for (t0, n) in tok_blocks(0, T):
                x = xt[bi % 2]
                bi += 1
                fw.dma("sp", x[:, :, 0:n], self.QKVC[:, t0:t0 + n].rearrange("(k p) t -> p k t", p=128), reads=[self.QKVC], writes=[x])
                fw.op("act", lambda h, x=x: h.activation(out=sq[:, :, 0:n], in_=x[:, :, 0:n], func=AF.Square), reads=[x], writes=[sq])
                for hf in range(2):
                    ps = self.ps()
                    fw.mm(ps[:, 0:n], [(self.ones_bf[:], sq[:, hf * 4 + k, 0:n]) for k in range(4)], reads=[sq, self.ones_bf], writes=[ps])
                    fw.op("act", lambda h, ps=ps: h.activation(out=rs[:, 0:n], in_=ps[:, 0:n], func=AF.Sqrt, scale=1.0 / 512, bias=self.eps_t[:]),
                          reads=[ps, self.eps_t], writes=[rs])
                    fw.op("dve", lambda h: h.reciprocal(out=rs[:, 0:n], in_=rs[:, 0:n]), reads=[rs], writes=[rs])
                    for k in range(4):
                        kk = hf * 4 + k
                        fw.op("dve", lambda h, x=x, kk=kk: h.scalar_tensor_tensor(
                            out=qkvn[:, kk, t0:t0 + n], in0=x[:, kk, 0:n], scalar=g[:, kk:kk + 1], in1=rs[:, 0:n],
                            op0=ALU.mult, op1=ALU.mult), reads=[x, rs, g], writes=[qkvn], disjoint=True)
            wq = fw.sb("wq", [128, 4, 1536], BF16)
            fw.dma("pool", wq[:], self.wq_up[l].rearrange("(k p) n -> p k n", p=128), writes=[wq])
            wqs = fw.sb("wqs", [128, 4, 8, 64], BF16)
            wq_heads = self.wq_up[l].rearrange("(k p) (h c) -> p k h c", p=128, c=192)
            for a in range(2):
                for s in range(2):
                    src0 = 128 + a * 32 + (1 - s) * 16
                    dst0 = a * 32 + s * 16
                    for k in range(4):
                        fw.dma("pool", wqs[:, k, :, dst0:dst0 + 16], wq_heads[:, k, :, src0:src0 + 16], writes=[wqs])
            wkv = fw.sb("wkv", [128, 4, 2048], BF16)
            fw.dma("pool", wkv[:], self.wkv_up[l].rearrange("(k p) n -> p k n", p=128), writes=[wkv])
            cos = fw.sb("cos", [64, NX], F32)
            sin = fw.sb("sin", [64, NX], F32)
            fw.dma("sp", cos[:], self.rope_cos[:], writes=[cos])
            fw.dma("sp", sin[:], self.rope_sin[:], writes=[sin])
            stq = [fw.sb("stq%d" % i, [128, 512], BF16) for i in range(3)]
            r1 = fw.sb("r1", [64, 512], F32)
            r2 = fw.sb("r2", [64, 512], F32)
            kpa = fw.sb("kpa", [64, 512], F32)
            kpb = fw.sb("kpb", [64, 512], F32)
            si = 0

            def rope(dst_ap, a_ap, b_ap, t0, n, a_buf, b_buf, dst_buf):
                fw.op("dve", lambda h: h.tensor_tensor(out=r1[:, 0:n], in0=a_ap, in1=cos[:, t0:t0 + n], op=ALU.mult),
                      reads=[a_buf, cos], writes=[r1])
                fw.op("dve", lambda h: h.tensor_tensor(out=r2[:, 0:n], in0=b_ap, in1=sin[:, t0:t0 + n], op=ALU.mult),
                      reads=[b_buf, sin], writes=[r2])
                fw.op("pool", lambda h: h.tensor_tensor(out=dst_ap, in0=r1[:, 0:n], in1=r2[:, 0:n], op=ALU.add),
                      reads=[r1, r2], writes=[dst_buf], disjoint=True)

            for (t0, n) in tok_blocks(0, T):
                lat = t0 < NX
                fw.dma("sp", kpa[:, 0:n], self.KPE[0:64, t0:t0 + n], reads=[self.KPE], writes=[kpa])
                if lat:
                    fw.dma("sp", kpb[:, 0:n], self.KPE[64:128, t0:t0 + n], reads=[self.KPE], writes=[kpb])
                    rope(kp[:, t0:t0 + n], kpa[:, 0:n], kpb[:, 0:n], t0, n, kpa, kpb, kp)
                else:
                    fw.op("pool", lambda h: h.tensor_copy(out=kp[:, t0:t0 + n], in_=kpa[:, 0:n]), reads=[kpa], writes=[kp], disjoint=True)
                for hd in range(8):
                    ps = self.ps()
                    fw.mm(ps[:, 0:n], [(wq[:, k, hd * 192:hd * 192 + 128], qkvn[:, k, t0:t0 + n]) for k in range(4)], reads=[wq, qkvn], writes=[ps])
                    st = stq[si % 3]
                    si += 1
                    fw.op("act", lambda h, ps=ps, st=st: h.copy(out=st[:, 0:n], in_=ps[:, 0:n]), reads=[ps], writes=[st])
                    fw.dma("sp", self.QN[hd, :, t0:t0 + n], st[:, 0:n], reads=[st], writes=[self.QN])
                    ps1 = self.ps()
                    fw.mm(ps1[0:64, 0:n], [(wq[:, k, hd * 192 + 128:hd * 192 + 192], qkvn[:, k, t0:t0 + n]) for k in range(4)], reads=[wq, qkvn], writes=[ps1])
                    st = stq[si % 3]
                    si += 1
                    if lat:
                        ps2 = self.ps()
                        fw.mm(ps2[0:64, 0:n], [(wqs[:, k, hd, :], qkvn[:, k, t0:t0 + n]) for k in range(4)], reads=[wqs, qkvn], writes=[ps2])
                        rope(st[0:64, 0:n], ps1[0:64, 0:n], ps2[0:64, 0:n], t0, n, ps1, ps2, st)
                    else:
                        fw.op("act", lambda h, ps1=ps1, st=st: h.copy(out=st[0:64, 0:n], in_=ps1[0:64, 0:n]), reads=[ps1], writes=[st])
                    fw.dma("sp", self.QP[hd, :, t0:t0 + n], st[0:64, 0:n], reads=[st], writes=[self.QP])
                    ps = self.ps()
                    fw.mm(ps[:, 0:n], [(wkv[:, k, hd * 256:hd * 256 + 128], qkvn[:, 4 + k, t0:t0 + n]) for k in range(4)], reads=[wkv, qkvn], writes=[ps])
                    st = stq[si % 3]
                    si += 1
                    fw.op("dve", lambda h, ps=ps, st=st: h.tensor_copy(out=st[:, 0:n], in_=ps[:, 0:n]), reads=[ps], writes=[st])
                    fw.dma("sp", self.KN[hd, :, t0:t0 + n], st[:, 0:n], reads=[st], writes=[self.KN])
            wkv_v = [wkv[:, k, :].rearrange("p (h two d) -> p h two d", two=2, d=128) for k in range(4)]
            for tt in range(NT):
                for hf in range(2):
                    ps = self.ps()
                    fw.mm(ps[:, :].rearrange("p (h d) -> p h d", d=128),
                          [(qkvn[:, 4 + k, tt * 128:(tt + 1) * 128], wkv_v[k][:, hf * 4:(hf + 1) * 4, 1, :]) for k in range(4)],
                          reads=[wkv, qkvn], writes=[ps])
                    st = stq[si % 3]
                    si += 1
                    fw.op("act" if hf else "dve", (lambda h, ps=ps, st=st: h.copy(out=st[:], in_=ps[:])) if hf else
                          (lambda h, ps=ps, st=st: h.tensor_copy(out=st[:], in_=ps[:])), reads=[ps], writes=[st])
                    fw.dma("sp", self.VT[tt * 128:(tt + 1) * 128, hf * 512:(hf + 1) * 512], st[:], reads=[st], writes=[self.VT])
          with fw.scope():
            self.attn_common_tiles()
            kn = [fw.sb("kn%d" % i, [128, T], BF16) for i in range(2)]
            vh = [fw.sb("vh%d" % i, [128, NT, 128], BF16) for i in range(2)]
            qn = [fw.sb("qn%d" % i, [128, 512], BF16) for i in range(2)]
            qp = [fw.sb("qp%d" % i, [64, 512], BF16) for i in range(2)]
            scale = 192.0 ** -0.5
            qi = 0
            for hd in range(8):
                k_ = kn[hd % 2]
                v_ = vh[hd % 2]
                fw.dma("sp", k_[:], self.KN[hd], reads=[self.KN], writes=[k_])
                fw.dma("sp", v_[:], self.VT[:, hd * 128:(hd + 1) * 128].rearrange("(t p) d -> p t d", p=128), reads=[self.VT], writes=[v_])
                qblocks = [(t0, n, list(range(NT))) for (t0, n) in tok_blocks(0, NX)]
                if not last:
                    qblocks.append((NX, c.NZ, list(range(NX // 128, NT))))
                for (t0, n, kts) in qblocks:
                    a, b = qn[qi % 2], qp[qi % 2]
                    qi += 1
                    fw.dma("sp", a[:, 0:n], self.QN[hd, :, t0:t0 + n], reads=[self.QN], writes=[a])
                    fw.dma("sp", b[:, 0:n], self.QP[hd, :, t0:t0 + n], reads=[self.QP], writes=[b])
                    self.qtile = a
                    self.qtile2 = b
                    self.attn_core([a[:, 0:n], b[:, 0:n]], [(k_, 0, 128), (kp, 0, 64)], v_, kts, n, scale,
                                   self.OBR[hd * 128:(hd + 1) * 128, t0:t0 + n], self.pts)

    def nattn(self, l):
        fw, c = self.fw, self.cfg
        T, NX = c.T, c.NX
        last = (l == c.L - 1)
        NT = T // 128
        NB = NX // 512
        rows = NX // 64
        with fw.scope():
            rp = fw.sb("rp", [8, 15 * 31], F32)
            fw.dma("sp", rp[:], self.na_rpb[l], writes=[rp])
            fw.op("act", lambda h: h.activation(out=rp[:], in_=rp[:], func=AF.Exp), reads=[rp], writes=[rp])
            rtv = self.RT[:, :].rearrange("h (i j) -> h i j", j=127)
            rp2 = fw.sb("rp2", [8, 15, 31], F32)
            fw.op("dve", lambda h: h.tensor_copy(out=rp2[:], in_=rp[:, :].rearrange("h (i j) -> h i j", j=31)[:, ::-1, :]), reads=[rp], writes=[rp2])
            fw.dma("sp", rtv[:, 4:19, 48:79], rp2[:], reads=[rp2], writes=[self.RT], disjoint=False)
            masks = fw.sb("namask", [128, NB * 8, 512], BF16)
            for b in range(NB):
                fw.dma("sp", masks[:, b * 8:(b + 1) * 8, :], self.na_mask[b].rearrange("t p q -> p t q"), writes=[masks])
            self.attn_common_tiles()
            kn = [fw.sb("nkn%d" % i, [128, T], BF16) for i in range(2)]
            vh = [fw.sb("nvh%d" % i, [128, NT, 128], BF16) for i in range(2)]
            qn = [fw.sb("nqn%d" % i, [128, 512], BF16) for i in range(2)]
            tbs = [fw.sb("tb%d" % i, [128, 8, 512], F32) for i in range(2)]
            scale = 128.0 ** -0.5
            qi = 0
            rt_t = self.RT.t.tensor if hasattr(self.RT.t, "tensor") else None
            for hd in range(8):
                k_ = kn[hd % 2]
                v_ = vh[hd % 2]
                tb = tbs[hd % 2]
                fw.dma("sp", k_[:], self.NAQK[1024 + hd * 128:1024 + (hd + 1) * 128, :], reads=[self.NAQK], writes=[k_])
                fw.dma("sp", v_[:], self.NAV[:, hd * 128:(hd + 1) * 128].rearrange("(t p) d -> p t d", p=128), reads=[self.NAV], writes=[v_])
                for t in range(8):
                    for a in range(2):
                        base = hd * 23 * 127 + (15 - 2 * t - a) * 127
                        src = bass.AP(tensor=self.RT.t.tensor, offset=base, ap=[[1, 64], [127, 8], [1, 64]])
                        fw.dma("sp", tb[a * 64:(a + 1) * 64, t, :].rearrange("p (q r) -> p q r", r=64), src, reads=[self.RT], writes=[tb])
                for b in range(NB):
                    a = qn[qi % 2]
                    qi += 1
                    t0 = b * 512
                    fw.dma("sp", a[:, :], self.NAQK[hd * 128:(hd + 1) * 128, t0:t0 + 512], reads=[self.NAQK], writes=[a])
                    self.qtile = a
                    base_row = 8 * b - 4
                    kts = []
                    tmap = {}
                    for t in range(8):
                        r0 = base_row + 2 * t
                        if 0 <= r0 and r0 + 1 < rows:
                            kt = r0 // 2
                            kts.append(kt)
                            tmap[kt] = t
                    kts += list(range(NX // 128, NT))

                    def extra(kt, pt, tmap=tmap, b=b, tb=tb):
                        if kt not in tmap:
                            return
                        t = tmap[kt]
                        fw.op("dve", lambda h: h.tensor_tensor(out=pt[:, :].rearrange("p (q u) -> p q u", u=64),
                                                               in0=pt[:, :].rearrange("p (q u) -> p q u", u=64),
                                                               in1=tb[:, t, :].rearrange("p (q u) -> p q u", u=64)[:, :, ::-1], op=ALU.mult),
                              reads=[pt, tb], writes=[pt])
                        fw.op("pool", lambda h: h.tensor_tensor(out=pt[:, :], in0=pt[:, :], in1=masks[:, b * 8 + t, :], op=ALU.mult),
                              reads=[pt, masks], writes=[pt])
                    self.attn_core([a[:, :]], [(k_, 0, 128)], v_, kts, 512, scale,
                                   self.OBR[1024 + hd * 128:1024 + (hd + 1) * 128, t0:t0 + 512], self.pts, extra=extra)
                if not last:
                    a = qn[qi % 2]
                    qi += 1
                    fw.dma("sp", a[:, 0:c.NZ], self.NAQK[hd * 128:(hd + 1) * 128, NX:T], reads=[self.NAQK], writes=[a])
                    self.qtile = a
                    self.attn_core([a[:, 0:c.NZ]], [(k_, 0, 128)], v_, list(range(NX // 128, NT)), c.NZ, scale,
                                   self.OBR[1024 + hd * 128:1024 + (hd + 1) * 128, NX:T], self.pts)

    def lru(self, l):
        fw, c = self.fw, self.cfg
        T, NX, NZ = c.T, c.NX, c.NZ
        last = (l == c.L - 1)
        segs = [(0, NX), (NX, T)]
        with fw.scope():
            cw = fw.sb("lcw", [128, 8, 4], F32)
            cb = fw.sb("lcb", [128, 8], F32)
            ba = fw.sb("lba", [128, 2, 8], F32)
            bx = fw.sb("lbx", [128, 2, 8], F32)
            lam = fw.sb("llam", [128, 2, 8], F32)
            fw.dma("sp", cw[:], self.lru_cw[:, l], writes=[cw])
            fw.dma("sp", cb[:], self.lru_cb[:, l], writes=[cb])
            fw.dma("sp", ba[:], self.lru_ba[:, l], writes=[ba])
            fw.dma("sp", bx[:], self.lru_bx[:, l], writes=[bx])
            fw.dma("sp", lam[:], self.lru_lam[:, l], writes=[lam])
            y = fw.sb("ly_", [128, 16], F32)
            z = fw.sb("lz_", [128, 16], F32)
            z2 = fw.sb("lz2_", [128, 16], F32)
            pacc = fw.sb("lpacc", [128, 16], F32)
            cA = fw.sb("cA", [128, 16], F32)
            lamf = lam[:, :, :].rearrange("p a b -> p (a b)")
            fw.op("act", lambda h: h.activation(out=y[:], in_=lamf, func=AF.Exp, scale=-1.0), reads=[lam], writes=[y])
            fw.op("dve", lambda h: h.tensor_scalar(out=z[:], in0=y[:], scalar1=2.0, scalar2=None, op0=ALU.add), reads=[y], writes=[z])
            fw.op("dve", lambda h: h.reciprocal(out=z[:], in_=z[:]), reads=[z], writes=[z])
            fw.op("dve", lambda h: h.tensor_tensor(out=z[:], in0=z[:], in1=y[:], op=ALU.mult), reads=[z, y], writes=[z])
            fw.op("dve", lambda h: h.tensor_tensor(out=z2[:], in0=z[:], in1=z[:], op=ALU.mult), reads=[z], writes=[z2])
            fw.op("dve", lambda h: h.tensor_scalar(out=pacc[:], in0=z2[:], scalar1=1.0 / 9, scalar2=1.0 / 7, op0=ALU.mult, op1=ALU.add), reads=[z2], writes=[pacc])
            for cst in (1.0 / 5, 1.0 / 3, 1.0):
                fw.op("dve", lambda h: h.tensor_tensor(out=pacc[:], in0=pacc[:], in1=z2[:], op=ALU.mult), reads=[pacc, z2], writes=[pacc])
                fw.op("dve", lambda h, cst=cst: h.tensor_scalar(out=pacc[:], in0=pacc[:], scalar1=cst, scalar2=None, op0=ALU.add), reads=[pacc], writes=[pacc])
            fw.op("dve", lambda h: h.tensor_tensor(out=pacc[:], in0=pacc[:], in1=z[:], op=ALU.mult), reads=[pacc, z], writes=[pacc])
            fw.op("dve", lambda h: h.tensor_scalar(out=cA[:], in0=pacc[:], scalar1=-16.0, scalar2=None, op0=ALU.mult), reads=[pacc], writes=[cA])
            lu = fw.sb("lu", [128, T], F32)
            u = fw.sb("u", [128, T], F32)
            ub = fw.sb("ub", [128, T], BF16)
            at = fw.sb("lat", [128, T], F32)
            bt = fw.sb("lbt", [128, T], F32)
            hs = fw.sb("lhs", [128, T], F32)
            hd_ = fw.sb("lhd", [128, T], F32)
            ly = fw.sb("lly", [128, T], F32)
            ob = fw.sb("lob", [128, T], BF16)
            wa = [fw.sb("lwa%d" % i, [128, 128], BF16) for i in range(2)]
            wx = [fw.sb("lwx%d" % i, [128, 128], BF16) for i in range(2)]
            rt = fw.sb("lrt", [128, 512], F32)
            it = fw.sb("lit", [128, 512], F32)
            for ch in range(8):
                fw.dma("sp", lu[:], self.LUY[ch * 128:(ch + 1) * 128, :], reads=[self.LUY], writes=[lu])
                fw.dma("sp", ly[:], self.LUY[1024 + ch * 128:1024 + (ch + 1) * 128, :], reads=[self.LUY], writes=[ly])
                fw.op("act", lambda h, ch=ch: h.activation(out=u[:], in_=lu[:], func=AF.Identity, scale=cw[:, ch, 2:3], bias=cb[:, ch:ch + 1]),
                      reads=[lu, cw, cb], writes=[u])
                for (s0, s1) in segs:
                    for j, sh in ((0, -2), (1, -1), (3, 1)):
                        if sh < 0:
                            o0, o1, i0, i1 = s0 - sh, s1, s0, s1 + sh
                        else:
                            o0, o1, i0, i1 = s0, s1 - sh, s0 + sh, s1
                        fw.op("dve", lambda h, ch=ch, j=j, o0=o0, o1=o1, i0=i0, i1=i1: h.scalar_tensor_tensor(
                            out=u[:, o0:o1], in0=lu[:, i0:i1], scalar=cw[:, ch, j:j + 1], in1=u[:, o0:o1], op0=ALU.mult, op1=ALU.add),
                            reads=[lu, u, cw], writes=[u])
                fw.op("pool", lambda h: h.tensor_copy(out=ub[:], in_=u[:]), reads=[u], writes=[ub])
                for d in range(2):
                    wa_, wx_ = wa[d], wx[d]
                    fw.dma("pool", wa_[:], self.lru_wa[l, d, ch], writes=[wa_])
                    fw.dma("pool", wx_[:], self.lru_wx[l, d, ch], writes=[wx_])
                    for (t0, n) in tok_blocks(0, T):
                        ps = self.ps()
                        fw.mm(ps[:, 0:n], [(wa_[:], ub[:, t0:t0 + n])], reads=[wa_, ub], writes=[ps])
                        fw.op("act", lambda h, ps=ps, d=d, ch=ch: h.activation(out=rt[:, 0:n], in_=ps[:, 0:n], func=AF.Sigmoid, bias=ba[:, d, ch:ch + 1]),
                              reads=[ps, ba], writes=[rt])
                        fw.op("act", lambda h, d=d, ch=ch: h.activation(out=at[:, t0:t0 + n], in_=rt[:, 0:n], func=AF.Exp, scale=cA[:, d * 8 + ch:d * 8 + ch + 1]),
                              reads=[rt, cA], writes=[at], disjoint=True)
                        ps2 = self.ps()
                        fw.mm(ps2[:, 0:n], [(wx_[:], ub[:, t0:t0 + n])], reads=[wx_, ub], writes=[ps2])
                        fw.op("act", lambda h, ps2=ps2, d=d, ch=ch: h.activation(out=it[:, 0:n], in_=ps2[:, 0:n], func=AF.Sigmoid, bias=bx[:, d, ch:ch + 1]),
                              reads=[ps2, bx], writes=[it])
                        fw.op("dve", lambda h: h.tensor_tensor(out=rt[:, 0:n], in0=at[:, t0:t0 + n], in1=at[:, t0:t0 + n], op=ALU.mult),
                              reads=[at], writes=[rt])
                        fw.op("act", lambda h: h.activation(out=rt[:, 0:n], in_=rt[:, 0:n], func=AF.Sqrt, scale=-1.0, bias=self.one_t[:]),
                              reads=[rt, self.one_t], writes=[rt])
                        fw.op("dve", lambda h: h.tensor_tensor(out=it[:, 0:n], in0=it[:, 0:n], in1=u[:, t0:t0 + n], op=ALU.mult),
                              reads=[it, u], writes=[it])
                        fw.op("dve", lambda h: h.tensor_tensor(out=bt[:, t0:t0 + n], in0=it[:, 0:n], in1=rt[:, 0:n], op=ALU.mult),
                              reads=[it, rt], writes=[bt], disjoint=True)
                    dst = hs if d == 0 else hd_
                    if d == 0:
                        fw.op("dve", lambda h, dst=dst: h.tensor_tensor_scan(out=dst[:, NX:T], data0=at[:, NX:T], data1=bt[:, NX:T], initial=0.0, op0=ALU.mult, op1=ALU.add),
                              reads=[at, bt], writes=[dst])
                        fw.op("dve", lambda h, dst=dst: h.tensor_tensor_scan(out=dst[:, 0:NX], data0=at[:, 0:NX], data1=bt[:, 0:NX], initial=dst[:, T - 1:T], op0=ALU.mult, op1=ALU.add),
                              reads=[at, bt, dst], writes=[dst])
                    else:
                        fw.op("dve", lambda h, dst=dst: h.tensor_tensor_scan(out=dst[:, NX:T][:, ::-1], data0=at[:, NX:T][:, ::-1], data1=bt[:, NX:T][:, ::-1], initial=0.0, op0=ALU.mult, op1=ALU.add),
                              reads=[at, bt], writes=[dst])
                        fw.op("dve", lambda h, dst=dst: h.tensor_tensor_scan(out=dst[:, 0:NX][:, ::-1], data0=at[:, 0:NX][:, ::-1], data1=bt[:, 0:NX][:, ::-1], initial=dst[:, NX:NX + 1], op0=ALU.mult, op1=ALU.add),
                              reads=[at, bt, dst], writes=[dst])
                fw.op("pool", lambda h: h.tensor_tensor(out=hs[:], in0=hs[:], in1=hd_[:], op=ALU.add), reads=[hs, hd_], writes=[hs])
                fw.op("dve", lambda h: h.tensor_tensor(out=hd_[:], in0=ly[:], in1=ly[:], op=ALU.mult), reads=[ly], writes=[hd_])
                fw.op("dve", lambda h: h.tensor_scalar(out=hd_[:], in0=hd_[:], scalar1=0.044715, scalar2=1.0, op0=ALU.mult, op1=ALU.add), reads=[hd_], writes=[hd_])
                fw.op("dve", lambda h: h.tensor_tensor(out=hd_[:], in0=hd_[:], in1=ly[:], op=ALU.mult), reads=[hd_, ly], writes=[hd_])
                fw.op("act", lambda h: h.activation(out=hd_[:], in_=hd_[:], func=AF.Sigmoid, scale=1.5957691216057308), reads=[hd_], writes=[hd_])
                fw.op("dve", lambda h: h.tensor_tensor(out=hd_[:], in0=hd_[:], in1=ly[:], op=ALU.mult), reads=[hd_, ly], writes=[hd_])
                fw.op("dve", lambda h: h.tensor_tensor(out=ob[:], in0=hd_[:], in1=hs[:], op=ALU.mult), reads=[hd_, hs], writes=[ob])
                fw.dma("sp", self.OBR[2048 + ch * 128:2048 + (ch + 1) * 128, :], ob[:], reads=[ob], writes=[self.OBR])


BIG = 30000.0


class Builder4(Builder3):
    def __init__(self, cfg):
        Builder3.__init__(self, cfg)
        fw, c = self.fw, cfg
        L = c.L
        di = lambda name, shape, dt=F32: fw.dram(name, shape, dt, kind="ExternalInput")
        self.dn_cw = di("dn_cw", [128, L, 24, 4])
        self.dn_alog = di("dn_alog", [128, L, 16])
        self.dn_dtb = di("dn_dtb", [128, L, 16])
        self.dn_ng = di("dn_ng", [128, L])
        self.dn_const = di("dn_const", [64, 7, 64])

    def prologue(self):
        Builder3.prologue(self)
        fw = self.fw
        self.one_t = fw.sb("one_t", [128, 1], F32)
        fw.op("pool", lambda h: h.memset(self.one_t[:], 1.0), writes=[self.one_t])
        with fw.scope():
            zt = fw.sb("zt", [8, 23 * 127], F32)
            fw.op("pool", lambda h: h.memset(zt[:], 0.0), writes=[zt])
            fw.dma("sp", self.RT[:, :], zt[:], reads=[zt], writes=[self.RT], disjoint=False)
        self.dnc = fw.sb("dnc", [64, 7, 64], F32)
        fw.dma("sp", self.dnc[:], self.dn_const[:], writes=[self.dnc])

    def mixers(self, l):
        sk = getattr(self.cfg, "skip", ())
        if "mla" not in sk: self.mla(l)
        if "nattn" not in sk: self.nattn(l)
        if "lru" not in sk: self.lru(l)
        if "deltanet" not in sk: self.deltanet(l)

    def merge(self, l):
        if "merge" not in getattr(self.cfg, "skip", ()): Builder3.merge(self, l)

    def moe(self, l):
        if "moe" not in getattr(self.cfg, "skip", ()): Builder3.moe(self, l)

    def deltanet(self, l):
        fw, c = self.fw, self.cfg
        T, NX, NZ = c.T, c.NX, c.NZ
        NCH = T // 64
        NCX = NX // 64
        segs = [(0, NX), (NX, T)]
        dnc = self.dnc
        I64 = dnc[:, 6, :]
        ones64 = self.ones_f[0:64, 0:64]
        with fw.scope():
            cw = fw.sb("dcw", [128, 24, 4], F32)
            fw.dma("sp", cw[:], self.dn_cw[:, l], writes=[cw])
            alog = fw.sb("dalog", [64, 16], F32)
            dtb = fw.sb("ddtb", [64, 16], F32)
            ng = fw.sb("dng", [128, c.L], F32)
            fw.dma("sp", alog[:], self.dn_alog[0:64, l, :], writes=[alog])
            fw.dma("sp", dtb[:], self.dn_dtb[0:64, l, :], writes=[dtb])
            fw.dma("sp", ng[:], self.dn_ng[:, :], writes=[ng])
            gat = fw.sb("gat", [64, NCH, 32], F32)
            fw.dma("sp", gat[:], self.GB[:, :].rearrange("(n c) f -> c n f", c=64), reads=[self.GB], writes=[gat])
            g_all = fw.sb("g_all", [64, NCH, 16], F32)
            b_all = fw.sb("b_all", [64, NCH, 16], F32)
            fw.op("act", lambda h: h.activation(out=alog[:], in_=alog[:], func=AF.Exp), reads=[alog], writes=[alog])
            fw.op("dve", lambda h: h.tensor_tensor(out=g_all[:], in0=gat[:, :, 0:16], in1=dtb[:].unsqueeze(1).to_broadcast([64, NCH, 16]), op=ALU.add),
                  reads=[gat, dtb], writes=[g_all])
            fw.op("act", lambda h: h.activation(out=g_all[:], in_=g_all[:], func=AF.Exp), reads=[g_all], writes=[g_all])
            fw.op("act", lambda h: h.activation(out=g_all[:], in_=g_all[:], func=AF.Ln, bias=self.one_t[0:64, :]), reads=[g_all, self.one_t], writes=[g_all])
            fw.op("dve", lambda h: h.scalar_tensor_tensor(out=g_all[:], in0=g_all[:], scalar=-1.0, in1=alog[:].unsqueeze(1).to_broadcast([64, NCH, 16]),
                                                          op0=ALU.mult, op1=ALU.mult), reads=[g_all, alog], writes=[g_all])
            fw.op("act", lambda h: h.activation(out=b_all[:], in_=gat[:, :, 16:32], func=AF.Sigmoid), reads=[gat], writes=[b_all])
            xin = fw.sb("dxin", [128, T], F32)
            qkv = [fw.sb("dqkv%d" % i, [128, T], F32) for i in range(3)]
            odT = fw.sb("odT", [128, T], F32)
            odT2 = fw.sb("odT2", [128, T], F32)
            S2 = fw.sb("dS2", [128, 128], F32)
            sqb = fw.sb("dsq", [128, 512], BF16)
            rsb = fw.sb("drs", [128, 512], F32)
            S1 = fw.sb("dS", [128, 128], F32)
            S = S1
            NB = 8
            tm = [fw.sb("dtm%d" % i, [64, NB, 128], F32) for i in range(3)]
            Kg = fw.sb("dKg", [64, NB, 128], F32)
            Vb = fw.sb("dVb", [64, NB, 128], F32)
            Kd = fw.sb("dKd", [64, NB, 128], F32)
            sm = {nm: fw.sb("d" + nm, [64, NB], F32) for nm in ("gcs", "egc", "ekd", "bge", "ngc")}
            gend = fw.sb("dgend", [128, NB], F32)
            T64 = {nm: fw.sb("d" + nm, [64, NB, 64], F32) for nm in
                   ("Dg", "GBC", "NGT", "Einc", "Lm", "Ai", "Xa", "XTa", "Xb", "XTb", "AiT", "R", "R2")}
            qdT = fw.sb("dqdT", [128, NB, 64], F32)
            nwkT = fw.sb("dnwkT", [128, NB, 64], F32)
            vn = [fw.sb("dvn%d" % i, [64, 128], F32) for i in range(2)]
            obf = fw.sb("dobf", [128, 512], BF16)
            dzt = fw.sb("ddzt", [128, 512], F32)
            vni = 0

            for hd in range(8):
                for part in range(3):
                    rc = part * 8 + hd
                    y = qkv[part]
                    fw.dma("sp", xin[:], self.DQKV[rc * 128:(rc + 1) * 128, :], reads=[self.DQKV], writes=[xin])
                    fw.op("act", lambda h, rc=rc, y=y: h.activation(out=y[:], in_=xin[:], func=AF.Identity, scale=cw[:, rc, 2:3]),
                          reads=[xin, cw], writes=[y])
                    for (s0, s1) in segs:
                        for j, sh in ((0, -2), (1, -1), (3, 1)):
                            if sh < 0:
                                o0, o1, i0, i1 = s0 - sh, s1, s0, s1 + sh
                            else:
                                o0, o1, i0, i1 = s0, s1 - sh, s0 + sh, s1
                            fw.op("dve", lambda h, rc=rc, j=j, o0=o0, o1=o1, i0=i0, i1=i1, y=y: h.scalar_tensor_tensor(
                                out=y[:, o0:o1], in0=xin[:, i0:i1], scalar=cw[:, rc, j:j + 1], in1=y[:, o0:o1], op0=ALU.mult, op1=ALU.add),
                                reads=[xin, y, cw], writes=[y])
                    fw.op("act", lambda h, y=y: h.activation(out=y[:], in_=y[:], func=AF.Silu), reads=[y], writes=[y])
                    if part < 2:
                        for (t0, n) in tok_blocks(0, T):
                            fw.op("act", lambda h, y=y: h.activation(out=sqb[:, 0:n], in_=y[:, t0:t0 + n], func=AF.Square), reads=[y], writes=[sqb])
                            ps = self.ps()
                            fw.mm(ps[:, 0:n], [(self.ones_bf[:], sqb[:, 0:n])], reads=[sqb, self.ones_bf], writes=[ps])
                            fw.op("act", lambda h, ps=ps: h.activation(out=rsb[:, 0:n], in_=ps[:, 0:n], func=AF.Sqrt, bias=self.eps_t[:]),
                                  reads=[ps, self.eps_t], writes=[rsb])
                            fw.op("dve", lambda h: h.reciprocal(out=rsb[:, 0:n], in_=rsb[:, 0:n]), reads=[rsb], writes=[rsb])
                            sc = (128.0 ** -0.5) if part == 0 else 1.0
                            fw.op("dve", lambda h, y=y, sc=sc: h.scalar_tensor_tensor(
                                out=y[:, t0:t0 + n], in0=y[:, t0:t0 + n], scalar=sc, in1=rsb[:, 0:n], op0=ALU.mult, op1=ALU.mult),
                                reads=[y, rsb], writes=[y])
                qT, kT, vT = qkv
                for d in range(2):
                    TRI = dnc[:, d, :]
                    BIGM = dnc[:, 2 + d, :]
                    SL = dnc[:, 4 + d, :]
                    fw.op("pool", lambda h: h.memset(S[:], 0.0), writes=[S])
                    ctxb = [list(range(NCX, NCH))]
                    latb = [list(range(b0, min(b0 + NB, NCX))) for b0 in range(0, NCX, NB)]
                    batches = ctxb + latb if d == 0 else ctxb + latb[::-1]
                    STG = getattr(c, "dn_stage", 9)
                    for chs in (batches if STG >= 1 else []):
                        nb = len(chs)
                        c0 = chs[0]
                        tk0 = c0 * 64
                        col = d * 8 + hd
                        gcol = g_all[:, c0:c0 + nb, col]
                        bcol = b_all[:, c0:c0 + nb, col]
                        gbufs = [g_all, b_all]
                        for part in range(3):
                            src = qkv[part]
                            for i0 in range(0, nb, 4):
                                ps = self.ps()
                                k4 = min(4, nb - i0)
                                fw.ops("pe", [lambda h, i=i, ps=ps, src=src: h.transpose(
                                    ps[0:64, (i - i0) * 128:(i - i0 + 1) * 128], src[:, tk0 + i * 64:tk0 + (i + 1) * 64], self.ident[:])
                                    for i in range(i0, i0 + k4)], reads=[src, self.ident], writes=[ps])
                                eng = "act" if (i0 // 4) % 2 == 0 else "dve"
                                if eng == "act":
                                    fw.op("act", lambda h, ps=ps, part=part, i0=i0, k4=k4: h.copy(
                                        out=tm[part][:, i0:i0 + k4, :], in_=ps[0:64, 0:k4 * 128].rearrange("p (a b) -> p a b", b=128)),
                                        reads=[ps], writes=[tm[part]], disjoint=True)
                                else:
                                    fw.op("dve", lambda h, ps=ps, part=part, i0=i0, k4=k4: h.tensor_copy(
                                        out=tm[part][:, i0:i0 + k4, :], in_=ps[0:64, 0:k4 * 128].rearrange("p (a b) -> p a b", b=128)),
                                        reads=[ps], writes=[tm[part]], disjoint=True)
                        Qm, Km, Vm = tm
                        ps = self.ps()
                        fw.mm(ps[0:64, 0:nb], [(TRI, gcol)], reads=[dnc] + gbufs, writes=[ps])
                        ps2 = self.ps()
                        fw.mm(ps2[:, 0:nb], [(self.ones_f[0:64, :], gcol)], reads=[self.ones_f] + gbufs, writes=[ps2])
                        fw.op("dve", lambda h, ps=ps: h.tensor_copy(out=sm["gcs"][:, 0:nb], in_=ps[0:64, 0:nb]), reads=[ps], writes=[sm["gcs"]])
                        fw.op("act", lambda h, ps=ps: h.activation(out=sm["egc"][:, 0:nb], in_=ps[0:64, 0:nb], func=AF.Exp), reads=[ps], writes=[sm["egc"]])
                        fw.op("dve", lambda h, ps2=ps2: h.tensor_tensor(out=sm["ekd"][:, 0:nb], in0=ps2[0:64, 0:nb], in1=sm["gcs"][:, 0:nb], op=ALU.subtract),
                              reads=[ps2, sm["gcs"]], writes=[sm["ekd"]])
                        fw.op("act", lambda h: h.activation(out=sm["ekd"][:, 0:nb], in_=sm["ekd"][:, 0:nb], func=AF.Exp), reads=[sm["ekd"]], writes=[sm["ekd"]])
                        fw.op("act", lambda h, ps2=ps2: h.activation(out=gend[:, 0:nb], in_=ps2[:, 0:nb], func=AF.Exp), reads=[ps2], writes=[gend])
                        fw.op("dve", lambda h: h.tensor_tensor(out=sm["bge"][:, 0:nb], in0=sm["egc"][:, 0:nb], in1=bcol, op=ALU.mult),
                              reads=[sm["egc"]] + gbufs, writes=[sm["bge"]])
                        bc = lambda ap_, w: ap_.unsqueeze(2).to_broadcast([64, nb, w])
                        fw.op("dve", lambda h: h.tensor_tensor(out=Kg[:, 0:nb, :], in0=Km[:, 0:nb, :], in1=bc(sm["bge"][:, 0:nb], 128), op=ALU.mult),
                              reads=[Km, sm["bge"]], writes=[Kg])
                        fw.op("pool", lambda h: h.tensor_tensor(out=Vb[:, 0:nb, :], in0=Vm[:, 0:nb, :], in1=bc(bcol, 128), op=ALU.mult),
                              reads=[Vm] + gbufs, writes=[Vb])
                        fw.op("pool", lambda h: h.tensor_tensor(out=Kd[:, 0:nb, :], in0=Km[:, 0:nb, :], in1=bc(sm["ekd"][:, 0:nb], 128), op=ALU.mult),
                              reads=[Km, sm["ekd"]], writes=[Kd])
                        Ib = I64.unsqueeze(1).to_broadcast([64, nb, 64])
                        Dg, GBC, NGT, Einc, Lm, Ai, AiT, R = (T64[k] for k in ("Dg", "GBC", "NGT", "Einc", "Lm", "Ai", "AiT", "R"))
                        fw.op("dve", lambda h: h.tensor_tensor(out=Dg[:, 0:nb, :], in0=Ib, in1=bc(sm["egc"][:, 0:nb], 64), op=ALU.mult),
                              reads=[dnc, sm["egc"]], writes=[Dg])
                        psq = self.ps()
                        for i in range(nb):
                            fw.mm(psq[:, i * 64:(i + 1) * 64], [(Qm[:, i, :], Dg[:, i, :])], reads=[Qm, Dg], writes=[psq])
                        fw.op("act", lambda h, psq=psq: h.copy(out=qdT[:, 0:nb, :], in_=psq[:, 0:nb * 64].rearrange("p (a b) -> p a b", b=64)),
                              reads=[psq], writes=[qdT])
                        if STG < 2: continue
                        fw.op("pool", lambda h: h.tensor_copy(out=GBC[:, 0:nb, :], in_=bc(gcol, 64)), reads=gbufs, writes=[GBC])
                        fw.op("dve", lambda h: h.scalar_tensor_tensor(out=NGT[:, 0:nb, :], in0=TRI.unsqueeze(1).to_broadcast([64, nb, 64]), scalar=-1.0,
                                                                      in1=bc(gcol, 64), op0=ALU.mult, op1=ALU.mult),
                              reads=[dnc] + gbufs, writes=[NGT])
                        psD, psG, psQK = self.ps(), self.ps(), self.ps()
                        for i in range(nb):
                            tk = tk0 + i * 64
                            fw.mm(psD[0:64, i * 64:(i + 1) * 64], [(GBC[:, i, :], TRI), (NGT[:, i, :], ones64), (I64, BIGM)],
                                  reads=[GBC, NGT, dnc, self.ones_f], writes=[psD])
                            fw.mm(psG[0:64, i * 64:(i + 1) * 64], [(kT[:, tk:tk + 64], kT[:, tk:tk + 64])], reads=[kT], writes=[psG])
                            fw.mm(psQK[0:64, i * 64:(i + 1) * 64], [(qT[:, tk:tk + 64], kT[:, tk:tk + 64])], reads=[qT, kT], writes=[psQK])
                        v3 = lambda t_: t_[0:64, 0:nb * 64].rearrange("p (a b) -> p a b", b=64)
                        if STG < 2.1: continue
                        fw.op("act", lambda h: h.activation(out=Einc[:, 0:nb, :], in_=v3(psD), func=AF.Exp, scale=-1.0), reads=[psD], writes=[Einc])
                        fw.op("dve", lambda h: h.tensor_tensor(out=Lm[:, 0:nb, :], in0=Einc[:, 0:nb, :], in1=v3(psG), op=ALU.mult), reads=[Einc, psG], writes=[Lm])
                        fw.op("dve", lambda h: h.tensor_tensor(out=Lm[:, 0:nb, :], in0=Lm[:, 0:nb, :], in1=bc(bcol, 64), op=ALU.mult), reads=[Lm] + gbufs, writes=[Lm])
                        fw.op("pool", lambda h: h.tensor_tensor(out=Lm[:, 0:nb, :], in0=Lm[:, 0:nb, :], in1=SL.unsqueeze(1).to_broadcast([64, nb, 64]), op=ALU.mult),
                              reads=[Lm, dnc], writes=[Lm])
                        fw.op("dve", lambda h: h.tensor_tensor(out=Ai[:, 0:nb, :], in0=Einc[:, 0:nb, :], in1=v3(psQK), op=ALU.mult), reads=[Einc, psQK], writes=[Ai])
                        if STG < 2.2: continue
                        psM, psAT = self.ps(), self.ps()
                        for i in range(nb):
                            fw.ops("pe", [lambda h, i=i: h.transpose(psM[0:64, i * 64:(i + 1) * 64], Lm[:, i, :], self.ident[0:64, 0:64])],
                                   reads=[Lm, self.ident], writes=[psM])
                            fw.ops("pe", [lambda h, i=i: h.transpose(psAT[0:64, i * 64:(i + 1) * 64], Ai[:, i, :], self.ident[0:64, 0:64])],
                                   reads=[Ai, self.ident], writes=[psAT])
                        X, XT = T64["Xa"], Lm
                        if STG < 2.3: continue
                        fw.op("act", lambda h: h.copy(out=X[:, 0:nb, :], in_=v3(psM)), reads=[psM], writes=[X])
                        if STG < 2.4: continue
                        fw.op("act", lambda h: h.copy(out=AiT[:, 0:nb, :], in_=v3(psAT)), reads=[psAT], writes=[AiT])
                        if STG < 2.5: continue
                        fw.op("dve", lambda h: h.tensor_tensor(out=R[:, 0:nb, :], in0=Ib, in1=X[:, 0:nb, :], op=ALU.subtract),
                              reads=[dnc, X], writes=[R])
                        if STG < 3: continue
                        pp = [(T64["Xb"], T64["XTb"]), (T64["Xa"], T64["XTa"])]
                        for it in range(5):
                            Xn, XnT = pp[it % 2]
                            psX, psXT = self.ps(), self.ps()
                            for i in range(nb):
                                if it < 4:
                                    fw.mm(psX[0:64, i * 64:(i + 1) * 64], [(XT[:, i, :], X[:, i, :])], reads=[XT, X], writes=[psX])
                                fw.mm(psXT[0:64, i * 64:(i + 1) * 64], [(X[:, i, :], XT[:, i, :])], reads=[XT, X], writes=[psXT])
                            if it < 4:
                                fw.op("act", lambda h, Xn=Xn, psX=psX: h.copy(out=Xn[:, 0:nb, :], in_=v3(psX)), reads=[psX], writes=[Xn])
                            fw.op("dve", lambda h, XnT=XnT, psXT=psXT: h.tensor_copy(out=XnT[:, 0:nb, :], in_=v3(psXT)), reads=[psXT], writes=[XnT])
                            psU = self.ps()
                            for i in range(nb):
                                fw.mm(psU[0:64, i * 64:(i + 1) * 64], [(XnT[:, i, :], R[:, i, :])], reads=[XnT, R], writes=[psU])
                            Rn = T64["R2"] if R is T64["R"] else T64["R"]
                            fw.op("dve", lambda h, psU=psU, Rn=Rn, R=R: h.tensor_tensor(out=Rn[:, 0:nb, :], in0=R[:, 0:nb, :], in1=v3(psU), op=ALU.add), reads=[R, psU], writes=[Rn])
                            R = Rn
                            X, XT = Xn, XnT
                        psW = self.ps()
                        for i in range(nb):
                            fw.mm(psW[:, i * 64:(i + 1) * 64], [(Kg[:, i, :], R[:, i, :])], reads=[Kg, R], writes=[psW])
                        fw.op("act", lambda h, psW=psW: h.activation(out=nwkT[:, 0:nb, :], in_=psW[:, 0:nb * 64].rearrange("p (a b) -> p a b", b=64),
                                                                     func=AF.Identity, scale=-1.0), reads=[psW], writes=[nwkT])
                        if STG < 4: continue
                        order = range(nb) if d == 0 else range(nb - 1, -1, -1)
                        for i in order:
                            tk = tk0 + i * 64
                            psv = self.ps()
                            fw.mm(psv[0:64, 0:128], [(R[:, i, :], Vb[:, i, :]), (nwkT[:, i, :], S[:])], reads=[R, Vb, nwkT, S], writes=[psv])
                            v_ = vn[vni % 2]
                            vni += 1
                            fw.op("act", lambda h, psv=psv, v_=v_: h.copy(out=v_[:], in_=psv[0:64, 0:128]), reads=[psv], writes=[v_])
                            pso = self.ps()
                            fw.mm(pso[:, 0:64], [(S[:], qdT[:, i, :]), (v_[:], AiT[:, i, :])], reads=[S, qdT, v_, AiT], writes=[pso])
                            od_ = odT if d == 0 else odT2
                            fw.op("act", lambda h, pso=pso, tk=tk, od_=od_: h.copy(out=od_[:, tk:tk + 64], in_=pso[:, 0:64]),
                                  reads=[pso], writes=[od_], disjoint=True)
                            psS = self.ps()
                            fw.mm(psS[:, 0:128], [(Kd[:, i, :], v_[:])], reads=[Kd, v_], writes=[psS])
                            Sn = S2 if S is S1 else S1
                            fw.op("dve", lambda h, psS=psS, i=i, S=S, Sn=Sn: h.scalar_tensor_tensor(out=Sn[:], in0=S[:], scalar=gend[:, i:i + 1], in1=psS[:, 0:128],
                                                                                                   op0=ALU.mult, op1=ALU.add), reads=[S, gend, psS], writes=[Sn])
                            S = Sn
                for (t0, n) in tok_blocks(0, T):
                    fw.dma("sp", dzt[:, 0:n], self.DZ[hd * 128:(hd + 1) * 128, t0:t0 + n], reads=[self.DZ], writes=[dzt])
                    fw.op("pool", lambda h: h.tensor_tensor(out=odT[:, t0:t0 + n], in0=odT[:, t0:t0 + n], in1=odT2[:, t0:t0 + n], op=ALU.add),
                          reads=[odT, odT2], writes=[odT])
                    fw.op("act", lambda h: h.activation(out=sqb[:, 0:n], in_=odT[:, t0:t0 + n], func=AF.Square), reads=[odT], writes=[sqb])
                    ps = self.ps()
                    fw.mm(ps[:, 0:n], [(self.ones_bf[:], sqb[:, 0:n])], reads=[sqb, self.ones_bf], writes=[ps])
                    fw.op("act", lambda h, ps=ps: h.activation(out=rsb[:, 0:n], in_=ps[:, 0:n], func=AF.Sqrt, scale=1.0 / 128, bias=self.eps_t[:]),
                          reads=[ps, self.eps_t], writes=[rsb])
                    fw.op("dve", lambda h: h.reciprocal(out=rsb[:, 0:n], in_=rsb[:, 0:n]), reads=[rsb], writes=[rsb])
                    fw.op("act", lambda h: h.activation(out=dzt[:, 0:n], in_=dzt[:, 0:n], func=AF.Silu), reads=[dzt], writes=[dzt])
                    fw.op("dve", lambda h: h.scalar_tensor_tensor(out=rsb[:, 0:n], in0=rsb[:, 0:n], scalar=ng[:, l:l + 1], in1=dzt[:, 0:n], op0=ALU.mult, op1=ALU.mult),
                          reads=[rsb, ng, dzt], writes=[rsb])
                    fw.op("dve", lambda h: h.tensor_tensor(out=obf[:, 0:n], in0=odT[:, t0:t0 + n], in1=rsb[:, 0:n], op=ALU.mult), reads=[odT, rsb], writes=[obf])
                    fw.dma("sp", self.OBR[3072 + hd * 128:3072 + (hd + 1) * 128, t0:t0 + n], obf[:, 0:n], reads=[obf], writes=[self.OBR])

BIGW_NAMES = ("ada_w", "w_in", "w_branch", "w_out", "exp_w1", "exp_w2")
MAXB = 250 * 1024 * 1024


def big_units(shape):
    L, C = shape[0], shape[-1]
    nsplit = 1
    while True:
        d1 = shape[1] // nsplit
        rows = d1
        for s_ in shape[2:-1]:
            rows *= s_
        if rows * C * 4 <= MAXB:
            break
        nsplit *= 2
    inner = [shape[1] // nsplit] + list(shape[2:-1])
    return nsplit, rows, C, inner


class GW(Buf):
    def __init__(self, name, views, nsplit, per):
        Buf.__init__(self, name)
        self.views, self.nsplit, self.per = views, nsplit, per

    def __getitem__(self, idx):
        l = idx[0]
        rest = tuple(idx[1:])
        if len(self.views[(l, 0)].shape) == 2:
            return self.views[(l, 0)][rest]
        d1 = rest[0]
        part = d1 // self.per
        return self.views[(l, part)][(d1 % self.per,) + rest[1:]]


class Builder5(Builder4):
    def __init__(self, cfg):
        Builder4.__init__(self, cfg)
        fw, nc = self.fw, self.nc
        self.gath = []
        for name in BIGW_NAMES:
            shape = list(getattr(self, name))
            L = shape[0]
            nsplit, Ru, C, inner = big_units(shape)
            assert Ru % 8 == 0
            nun = L * nsplit
            sh_in = fw.dram(name + "_sh", [nun * (Ru // 8), C], F32, kind="ExternalInput")
            views = {}
            units = []
            for l in range(L):
                for p_ in range(nsplit):
                    u = l * nsplit + p_
                    bounce = fw.dram("%s_b%d" % (name, u), [Ru // 8, C], F32)
                    G = fw.dram("%s_g%d" % (name, u), [Ru, C], F32)
                    units.append((u, bounce, G))
                    if len(inner) == 1:
                        views[(l, p_)] = G.t
                    else:
                        dims = " ".join("d%d" % i for i in range(len(inner)))
                        kw = {"d%d" % i: inner[i] for i in range(1, len(inner))}
                        views[(l, p_)] = G.t.rearrange("(%s) c -> %s c" % (dims, dims), **kw)
            self.gath.append((name, sh_in, units, Ru, C))
            setattr(self, name, GW(name, views, nsplit, inner[0]))

    def prologue(self):
        fw, nc = self.fw, self.nc
        cc_sem = fw.es.enter_context(nc.semaphore("cc_sem"))
        ncc = 0
        Q = fw.Q["pool"]
        for (name, sh_in, units, Ru, C) in self.gath:
            rs = Ru // 8
            for (u, bounce, G) in units:
                fw.dma("pool", bounce[:, :], sh_in[u * rs:(u + 1) * rs, :], reads=[sh_in], writes=[bounce])
            for (u, bounce, G) in units:
                fw._deps(Q.h, Q.seen, None, [bounce], [G], True)
                nc.gpsimd.collective_compute("AllGather", ALU.bypass, replica_groups=[list(range(8))],
                                             ins=[bounce[:, :]], outs=[G[:, :]]).then_inc(cc_sem)
                ncc += 1
                fw.n_instr += 1
                Q.h.wait_ge(cc_sem, ncc)
        Q.h.wait_ge(cc_sem, ncc)
        fw.barrier()
        Builder4.prologue(self)


def shard_weights(inp, r):
    out = {}
    for name in BIGW_NAMES:
        w = inp[name]
        nsplit, Ru, C, inner = big_units(list(w.shape))
        w2 = w.reshape(-1, 8, Ru // 8, C)[:, r]
        out[name + "_sh"] = np.ascontiguousarray(w2.reshape(-1, C))
    return out

import numpy as np
import ml_dtypes

D = 2048
BIG = 30000.0


def fm(v):
    v = np.asarray(v)
    return np.ascontiguousarray(np.moveaxis(v.reshape(v.shape[:-1] + (-1, 128)), -1, 0))


def rope_tables(NX):
    t = np.arange(NX)
    row = (t // 64).astype(np.float32)
    col = (t % 64).astype(np.float32)
    nf = 16
    inv_freq = (10000.0 ** (-np.arange(nf, dtype=np.float32) / nf)).astype(np.float32)
    ang = np.stack([row[:, None] * inv_freq, col[:, None] * inv_freq], axis=1)
    cos, sin = np.cos(ang).astype(np.float32), np.sin(ang).astype(np.float32)
    C = np.zeros((64, NX), np.float32)
    S = np.zeros((64, NX), np.float32)
    for a in range(2):
        for s in range(2):
            C[a * 32 + s * 16:a * 32 + s * 16 + 16] = cos[:, a, :].T
            S[a * 32 + s * 16:a * 32 + s * 16 + 16] = (sin[:, a, :].T) * (-1.0 if s == 0 else 1.0)
    return C, S


def na_masks(NX):
    rows = NX // 64
    NB = NX // 512
    wr = min(8, rows)
    M = np.zeros((NB, 8, 128, 512), np.float32)
    qr_l = np.arange(8)[:, None]
    qc = np.arange(64)[None, :]
    for b in range(NB):
        qr = 8 * b + qr_l
        rs = np.clip(qr - wr // 2, 0, rows - wr)
        cs = np.clip(qc - 8, 0, 64 - 16)
        for t in range(8):
            for kr_l in range(2):
                kr = 8 * b - 4 + 2 * t + kr_l
                if kr < 0 or kr >= rows:
                    continue
                rowok = (kr >= rs) & (kr < rs + wr)
                for kc in range(64):
                    colok = (kc >= cs) & (kc < cs + 16)
                    M[b, t, kr_l * 64 + kc] = (rowok & colok).astype(np.float32).reshape(512)
    return M.astype(ml_dtypes.bfloat16)


def dn_consts():
    c = np.zeros((64, 7, 64), np.float32)
    i = np.arange(64)
    c[:, 0, :] = (i[:, None] <= i[None, :])
    c[:, 1, :] = (i[:, None] >= i[None, :])
    c[:, 2, :] = BIG * (i[None, :] > i[:, None])
    c[:, 3, :] = BIG * (i[None, :] < i[:, None])
    c[:, 4, :] = (i[None, :] < i[:, None])
    c[:, 5, :] = (i[None, :] > i[:, None])
    c[:, 6, :] = np.eye(64)
    return c


def shared_inputs(inp, NX):
    L = inp["ada_w"].shape[0]
    E = inp["router_w"].shape[-1]
    f32 = np.float32
    out = {}
    out["ada_w"] = inp["ada_w"]
    out["ada_b"] = np.ascontiguousarray(fm(inp["ada_b"]))
    out["norm_g"] = np.ascontiguousarray(fm(np.stack([inp["norm_mix_g"], inp["norm_ffn_g"]], axis=1)))
    out["w_in"] = inp["w_in"]
    out["final_g"] = fm(inp["final_g"])
    out["w_branch"] = inp["w_branch"]
    out["w_out"] = inp["w_out"]
    out["router_w"] = inp["router_w"]
    out["router_b"] = np.ascontiguousarray(inp["router_b"][None])
    out["exp_w1"] = inp["exp_w1"]
    b1 = inp["exp_b1"].reshape(L, E, 5, 128, 2)
    out["exp_b1"] = np.ascontiguousarray(np.transpose(b1, (3, 0, 1, 4, 2)))
    out["exp_w2"] = inp["exp_w2"]
    out["exp_b2"] = inp["exp_b2"]
    out["ident"] = np.eye(128, dtype=f32)
    out["mla_g"] = np.ascontiguousarray(np.concatenate([fm(inp["mla_qn_g"]), fm(inp["mla_kvn_g"])], axis=-1))
    out["mla_wq_up"] = inp["mla_wq_up"]
    out["mla_wkv_up"] = inp["mla_wkv_up"]
    C, S = rope_tables(NX)
    out["rope_cos"], out["rope_sin"] = C, S
    out["na_rpb"] = np.ascontiguousarray(inp["na_rpb"].reshape(L, 8, 15 * 31))
    out["na_mask"] = na_masks(NX)
    cw = inp["lru_conv_w"]
    out["lru_cw"] = np.ascontiguousarray(np.transpose(fm(cw), (0, 1, 3, 2)))
    out["lru_cb"] = fm(inp["lru_conv_b"])
    out["lru_wa"] = inp["lru_wa"]
    out["lru_wx"] = inp["lru_wx"]
    out["lru_ba"] = fm(inp["lru_ba"])
    out["lru_bx"] = fm(inp["lru_bx"])
    out["lru_lam"] = fm(inp["lru_lam"])
    out["dn_cw"] = np.ascontiguousarray(np.transpose(fm(inp["dn_conv_w"]), (0, 1, 3, 2)))
    out["dn_alog"] = np.ascontiguousarray(np.broadcast_to(inp["dn_a_log"].reshape(1, L, 16), (128, L, 16))).astype(f32)
    out["dn_dtb"] = np.ascontiguousarray(np.broadcast_to(inp["dn_dt_bias"].reshape(1, L, 16), (128, L, 16))).astype(f32)
    out["dn_ng"] = np.ascontiguousarray(inp["dn_norm_g"].T)
    out["dn_const"] = dn_consts()
    return {k: np.ascontiguousarray(v) for k, v in out.items()}


def core_inputs(inp, b):
    xz = np.concatenate([inp["x"][b], inp["ctx"][b]], axis=0)
    return {
        "xT": np.ascontiguousarray(xz.T),
        "cc": np.ascontiguousarray(np.stack([fm(inp["c"][b]), fm(inp["c_ctx"])], axis=-1)),
    }

from concourse.bass_utils import run_bass_kernel_spmd


def kernel(**inputs):
    inp = {k: np.asarray(v) for k, v in inputs.items()}
    B, NX, _ = inp["x"].shape
    NZ = inp["ctx"].shape[1]
    L = inp["ada_w"].shape[0]
    E = inp["router_w"].shape[-1]
    cfg = Cfg(NX=NX, NZ=NZ, E=E, L=L)
    cfg.shard = True
    nc = Builder5(cfg).build()
    shared = shared_inputs(inp, NX)
    for name in ("ada_w", "w_in", "w_branch", "w_out", "exp_w1", "exp_w2"):
        shared.pop(name)
    in_maps = []
    for b in range(B):
        m = dict(shared)
        m.update(core_inputs(inp, b))
        m.update(shard_weights(inp, b))
        in_maps.append(m)
    res = run_bass_kernel_spmd(nc, in_maps, core_ids=list(range(B)))
    out = np.stack([np.ascontiguousarray(np.asarray(r["out"]).T) for r in res.results], axis=0)
    return out.astype(np.float32)
```
